# Optimizing a Trainium2 kernel written in Bass

```python
import math
import jax, jax.numpy as jnp
from jax import lax
import numpy as np

D_MODEL = 1024
BATCH = 8
SEQ = 4096
DEPTH = 1

CTX_LEN = 256
GRID_W = 64
HA = 8
DA = 64
HB = 8
NOPE = 128
ROPE = 64
VB = 128
Q_LORA = 256
KV_LORA = 256
N_KEYS = 128
N_EXPERTS = N_KEYS * N_KEYS
HP = 8
DK_HALF = 128
TOPK_HALF = 16
TOPK = 16
ROPE_BASE = 10000.0
Q_BLOCK = 128
TOK_BLOCK = 128
EPS = 1e-6
DEEPNORM_ALPHA = (2 * DEPTH) ** 0.25
DEEPNORM_BETA = (8 * DEPTH) ** -0.25
Q_A = HA * 2 * DA
GATES = 2 * D_MODEL
K_A = HA * 2 * DA
V_A = HA * 2 * DA
Q_COLS = Q_A + Q_LORA + GATES
KV_COLS = K_A + V_A + KV_LORA + ROPE
IN_COLS = Q_COLS + KV_COLS

kernel_name = "hybrid_diffattn_mla_peer_block"


def rms_norm(x, g):
    xf = x.astype(jnp.float32)
    y = xf * lax.rsqrt(jnp.mean(xf * xf, -1, keepdims=True) + EPS)
    return (y * g).astype(x.dtype)


def layer_norm(x, g, b):
    xf = x.astype(jnp.float32)
    mu = jnp.mean(xf, -1, keepdims=True)
    var = jnp.mean(jnp.square(xf - mu), -1, keepdims=True)
    return ((xf - mu) * lax.rsqrt(var + EPS) * g + b).astype(x.dtype)


def axial_rope(n, rot_dim):
    rows = n // GRID_W
    quarter = rot_dim // 4
    inv = ROPE_BASE ** (-jnp.arange(quarter, dtype=jnp.float32) / quarter)
    row_ang = jnp.arange(rows, dtype=jnp.float32)[:, None] * inv
    col_ang = jnp.arange(GRID_W, dtype=jnp.float32)[:, None] * inv
    ang = jnp.concatenate([
        jnp.broadcast_to(row_ang[:, None, :], (rows, GRID_W, quarter)),
        jnp.broadcast_to(col_ang[None, :, :], (rows, GRID_W, quarter))], -1).reshape(n, 2 * quarter)
    return jnp.cos(ang), jnp.sin(ang)


def apply_rope(x, cos, sin):
    half = x.shape[-1] // 2
    cos, sin = cos.astype(x.dtype), sin.astype(x.dtype)
    x1, x2 = x[..., :half], x[..., half:]
    return jnp.concatenate([x1 * cos - x2 * sin, x1 * sin + x2 * cos], -1)


def kv_side(h, w_in, w_kv_up, kv_norm_g, rope):
    B, N, _ = h.shape
    p = h @ w_in[:, Q_COLS:]
    k_a, v_a, kv_lat, k_rope = jnp.split(p, [K_A, K_A + V_A, K_A + V_A + KV_LORA], axis=-1)
    k_a = k_a.reshape(B, N, HA, 2, DA)
    v_a = v_a.reshape(B, N, HA, 2 * DA)
    kv = (rms_norm(kv_lat, kv_norm_g) @ w_kv_up).reshape(B, N, HB, NOPE + VB)
    k_nope, v_b = kv[..., :NOPE], kv[..., NOPE:]
    if rope is not None:
        cos, sin = rope
        k_a = apply_rope(k_a, cos[:, None, None, :], sin[:, None, None, :])
        k_rope = apply_rope(k_rope, cos, sin)
    return (k_a, v_a, k_nope, v_b, k_rope)


def q_side(h, w_in, w_q_up, q_norm_g, rope):
    B, N, _ = h.shape
    p = h @ w_in[:, :Q_COLS]
    q_a, q_lat, gates = jnp.split(p, [Q_A, Q_A + Q_LORA], axis=-1)
    q_a = q_a.reshape(B, N, HA, 2, DA)
    q_b = (rms_norm(q_lat, q_norm_g) @ w_q_up).reshape(B, N, HB, NOPE + ROPE)
    q_nope, q_rope = q_b[..., :NOPE], q_b[..., NOPE:]
    if rope is not None:
        cos, sin = rope
        q_a = apply_rope(q_a, cos[:, None, None, :], sin[:, None, None, :])
        q_rope = apply_rope(q_rope, cos[:, None, :], sin[:, None, :])
    return (q_a, q_nope, q_rope, gates)


def sweep(fn, *qs):
    B, N = qs[0].shape[:2]
    nb = N // Q_BLOCK
    blk = tuple(jnp.moveaxis(q.reshape(B, nb, Q_BLOCK, *q.shape[2:]), 1, 0) for q in qs)
    out = lax.map(lambda a: fn(*a), blk)
    return jnp.moveaxis(out, 0, 1).reshape(B, N, *out.shape[3:])


def diff_attention(q_a, k_a, v_a, lam, subln_g, lambda_init):
    scale = DA ** -0.5

    def block(qb):
        s = jnp.einsum('bqhmd,bkhmd->bhmqk', qb, k_a).astype(jnp.float32) * scale
        p = jax.nn.softmax(s, axis=-1)
        p = p[:, :, 0] - lam * p[:, :, 1]
        return jnp.einsum('bhqk,bkhe->bqhe', p.astype(v_a.dtype), v_a)

    o = sweep(block, q_a)
    return rms_norm(o, subln_g) * (1.0 - lambda_init)


def mla_attention(q_nope, q_rope, k_nope, k_rope, v_b):
    scale = (NOPE + ROPE) ** -0.5

    def block(qn, qr):
        s = (jnp.einsum('bqhd,bkhd->bhqk', qn, k_nope)
             + jnp.einsum('bqhr,bkr->bhqk', qr, k_rope)).astype(jnp.float32) * scale
        p = jax.nn.softmax(s, axis=-1)
        return jnp.einsum('bhqk,bkhe->bqhe', p.astype(v_b.dtype), v_b)

    return sweep(block, q_nope, q_rope)


def token_mixers(q, kv, lam, lambda_init, subln_g, w_pa, w_pb, w_out):
    q_a, q_nope, q_rope, gates = q
    k_a, v_a, k_nope, v_b, k_rope = kv
    B, N = gates.shape[:2]
    y_a = diff_attention(q_a, k_a, v_a, lam, subln_g, lambda_init).reshape(B, N, HA * 2 * DA)
    y_b = mla_attention(q_nope, q_rope, k_nope, k_rope, v_b).reshape(B, N, HB * VB)
    g_a = jax.nn.sigmoid(gates[..., :D_MODEL])
    g_b = jax.nn.sigmoid(gates[..., D_MODEL:])
    z = g_a * (y_a @ w_pa) + g_b * (y_b @ w_pb)
    return z @ w_out


def peer_ffn(h, w_pq, peer_keys, peer_u, peer_v):
    B, N, D = h.shape
    q = (h @ w_pq).reshape(B, N, HP, 2, DK_HALF)
    s = jnp.einsum('bnhmd,hmkd->bnhmk', q, peer_keys).astype(jnp.float32)
    s_top, i_top = lax.top_k(s, TOPK_HALF)
    cand = s_top[..., 0, :, None] + s_top[..., 1, None, :]
    cand_idx = i_top[..., 0, :, None] * N_KEYS + i_top[..., 1, None, :]
    g_s, pos = lax.top_k(cand.reshape(B, N, HP, TOPK_HALF * TOPK_HALF), TOPK)
    idx = jnp.take_along_axis(cand_idx.reshape(B, N, HP, TOPK_HALF * TOPK_HALF), pos, axis=-1)
    g = jax.nn.softmax(g_s, axis=-1)
    T = B * N
    nb = T // TOK_BLOCK
    xt = h.reshape(nb, TOK_BLOCK, D)
    it = idx.reshape(nb, TOK_BLOCK, HP * TOPK)
    gt = g.reshape(nb, TOK_BLOCK, HP * TOPK).astype(h.dtype)

    def block(args):
        xb, ib, gb = args
        u = peer_u[ib]
        v = peer_v[ib]
        a = jnp.einsum('td,tjd->tj', xb, u)
        return jnp.einsum('tj,tjd->td', jax.nn.gelu(a) * gb, v)

    return lax.map(block, (xt, it, gt)).reshape(B, N, D)


def setup_inputs(seed: int = 0) -> dict:
    key = jax.random.key(seed)
    ks = jax.random.split(key, 32)
    L = DEPTH

    def nrm(k, shape, scale):
        return jax.random.normal(k, shape, jnp.float32) * scale

    beta = DEEPNORM_BETA
    return {
        "x": nrm(ks[0], (BATCH, SEQ, D_MODEL), 1.0),
        "c": nrm(ks[1], (BATCH, D_MODEL), 1.0),
        "ctx": nrm(ks[2], (BATCH, CTX_LEN, D_MODEL), 1.0),
        "c_ctx": nrm(ks[3], (D_MODEL,), 1.0),
        "w_mod": nrm(ks[4], (L, D_MODEL, 6 * D_MODEL), 0.5 * D_MODEL ** -0.5),
        "b_mod": nrm(ks[5], (L, 6 * D_MODEL), 0.01),
        "w_in": nrm(ks[6], (L, D_MODEL, IN_COLS), D_MODEL ** -0.5),
        "w_q_up": nrm(ks[7], (L, Q_LORA, HB * (NOPE + ROPE)), Q_LORA ** -0.5),
        "q_norm_g": 1.0 + nrm(ks[8], (L, Q_LORA), 0.02),
        "w_kv_up": nrm(ks[9], (L, KV_LORA, HB * (NOPE + VB)), KV_LORA ** -0.5),
        "kv_norm_g": 1.0 + nrm(ks[10], (L, KV_LORA), 0.02),
        "lambda_q1": nrm(ks[11], (L, DA), 0.1),
        "lambda_k1": nrm(ks[12], (L, DA), 0.1),
        "lambda_q2": nrm(ks[13], (L, DA), 0.1),
        "lambda_k2": nrm(ks[14], (L, DA), 0.1),
        "subln_g": 1.0 + nrm(ks[15], (L, 2 * DA), 0.02),
        "w_pa": nrm(ks[16], (L, HA * 2 * DA, D_MODEL), beta * (HA * 2 * DA) ** -0.5),
        "w_pb": nrm(ks[17], (L, HB * VB, D_MODEL), beta * (HB * VB) ** -0.5),
        "w_out": nrm(ks[18], (L, D_MODEL, D_MODEL), beta * D_MODEL ** -0.5),
        "ln1_g": 1.0 + nrm(ks[19], (L, D_MODEL), 0.02),
        "ln1_b": nrm(ks[20], (L, D_MODEL), 0.01),
        "w_pq": nrm(ks[21], (L, D_MODEL, HP * 2 * DK_HALF), D_MODEL ** -0.5),
        "peer_keys": nrm(ks[22], (L, HP, 2, N_KEYS, DK_HALF), DK_HALF ** -0.5),
        "peer_u": nrm(ks[23], (L, N_EXPERTS, D_MODEL), D_MODEL ** -0.5),
        "peer_v": nrm(ks[24], (L, N_EXPERTS, D_MODEL), beta),
        "ln2_g": 1.0 + nrm(ks[25], (L, D_MODEL), 0.02),
        "ln2_b": nrm(ks[26], (L, D_MODEL), 0.01),
    }


def reference(x, c, ctx, c_ctx, w_mod, b_mod, w_in, w_q_up, q_norm_g, w_kv_up, kv_norm_g,
              lambda_q1, lambda_k1, lambda_q2, lambda_k2, subln_g, w_pa, w_pb, w_out,
              ln1_g, ln1_b, w_pq, peer_keys, peer_u, peer_v, ln2_g, ln2_b):
    rope = axial_rope(x.shape[1], DA)
    alpha = DEEPNORM_ALPHA
    for l in range(DEPTH):
        last = l == DEPTH - 1
        lambda_init = 0.8 - 0.6 * math.exp(-0.3 * l)
        mod = jax.nn.silu(c) @ w_mod[l] + b_mod[l]
        sh1, sc1, gt1, sh2, sc2, gt2 = [m[:, None, :] for m in jnp.split(mod, 6, axis=-1)]
        mod_c = jax.nn.silu(c_ctx) @ w_mod[l] + b_mod[l]
        csh1, csc1, cgt1, csh2, csc2, cgt2 = jnp.split(mod_c, 6, axis=-1)
        lam = (jnp.exp(jnp.sum(lambda_q1[l] * lambda_k1[l]).astype(jnp.float32))
               - jnp.exp(jnp.sum(lambda_q2[l] * lambda_k2[l]).astype(jnp.float32)) + lambda_init)

        h = x * (1.0 + sc1) + sh1
        hc = ctx * (1.0 + csc1) + csh1
        kv_c = kv_side(hc, w_in[l], w_kv_up[l], kv_norm_g[l], None)
        kv_x = kv_side(h, w_in[l], w_kv_up[l], kv_norm_g[l], rope)
        kv_all = tuple(jnp.concatenate([a, b], axis=1) for a, b in zip(kv_c, kv_x))
        q_x = q_side(h, w_in[l], w_q_up[l], q_norm_g[l], rope)
        y = token_mixers(q_x, kv_all, lam, lambda_init, subln_g[l], w_pa[l], w_pb[l], w_out[l])
        x = layer_norm(alpha * x + gt1 * y, ln1_g[l], ln1_b[l])
        if not last:
            q_c = q_side(hc, w_in[l], w_q_up[l], q_norm_g[l], None)
            yc = token_mixers(q_c, kv_c, lam, lambda_init, subln_g[l], w_pa[l], w_pb[l], w_out[l])
            ctx = layer_norm(alpha * ctx + cgt1 * yc, ln1_g[l], ln1_b[l])

        h = x * (1.0 + sc2) + sh2
        x = layer_norm(alpha * x + gt2 * peer_ffn(h, w_pq[l], peer_keys[l], peer_u[l], peer_v[l]),
                       ln2_g[l], ln2_b[l])
        if not last:
            hc = ctx * (1.0 + csc2) + csh2
            ctx = layer_norm(alpha * ctx + cgt2 * peer_ffn(hc, w_pq[l], peer_keys[l], peer_u[l], peer_v[l]),
                             ln2_g[l], ln2_b[l])
    return x
```

```python
import numpy as np
import concourse.bass as bass
import concourse.mybir as mybir

F32 = mybir.dt.float32
BF16 = mybir.dt.bfloat16
U32 = mybir.dt.uint32
ALU = mybir.AluOpType
ACTF = mybir.ActivationFunctionType
AX = mybir.AxisListType


class Dep:
    __slots__ = ("w", "r")

    def __init__(self):
        self.w = {}
        self.r = {}


class FW:
    NDMA = 24
    SELF_SYNC = True
    NOSELF = ("act",)

    def __init__(self, nc, stack):
        self.nc = nc
        self.engs = {"pe": nc.tensor, "act": nc.scalar, "dve": nc.vector,
                     "pool": nc.gpsimd, "sp": nc.sync}
        self.sems = {}
        self.count = {}
        self.known = {}
        self.hist = {}
        self.prog = {k: [] for k in self.engs}
        for k in self.engs:
            self.sems[k] = stack.enter_context(nc.semaphore("s_" + k))
            self.count[k] = 0
            self.known[k] = {}
            self.hist[k] = {}
        self.dma_pool = {}
        for q in ("sp", "pool", "act"):
            lst = []
            for i in range(self.NDMA):
                key = "d_%s_%d" % (q, i)
                self.sems[key] = stack.enter_context(nc.semaphore(key))
                self.count[key] = 0
                self.hist[key] = {}
                lst.append(key)
            self.dma_pool[q] = [lst, 0]

    def _learn(self, e, key, val):
        kn = self.known[e]
        if kn.get(key, 0) < val:
            kn[key] = val
        snap = self.hist[key].get(val)
        if snap:
            for k2, v2 in snap.items():
                if kn.get(k2, 0) < v2:
                    kn[k2] = v2

    def _waits(self, e, reads, writes, extra=()):
        need = {}
        for d in reads:
            for k, v in d.w.items():
                if need.get(k, 0) < v:
                    need[k] = v
        for d in writes:
            for k, v in d.w.items():
                if need.get(k, 0) < v:
                    need[k] = v
            for k, v in d.r.items():
                if need.get(k, 0) < v:
                    need[k] = v
        for k, v in extra:
            if need.get(k, 0) < v:
                need[k] = v
        kn = self.known[e]
        for k, v in need.items():
            if k == e and (e == "pe" or e in self.NOSELF):
                continue
            if kn.get(k, 0) >= v:
                continue
            eng = self.engs[e]
            sem = self.sems[k]
            self.prog[e].append((lambda eng=eng, sem=sem, v=v: eng.wait_ge(sem, v)))
            self._learn(e, k, v)

    def op(self, e, fn, reads=(), writes=()):
        self._waits(e, reads, writes)
        self.count[e] += 1
        c = self.count[e]
        eng = self.engs[e]
        sem = self.sems[e]
        self.prog[e].append((lambda eng=eng, sem=sem, fn=fn: fn(eng).then_inc(sem, 1)))
        self.hist[e][c] = dict(self.known[e])
        for d in reads:
            d.r[e] = c
        for d in writes:
            d.w[e] = c

    def dma(self, q, out, in_, reads=(), writes=(), **kw):
        lst, idx = self.dma_pool[q]
        key = lst[idx % len(lst)]
        self.dma_pool[q][1] = idx + 1
        prev = self.count[key]
        extra = [(key, prev)] if prev > 0 else []
        self._waits(q, reads, writes, extra)
        self.count[key] = prev + 16
        v = prev + 16
        eng = self.engs[q]
        sem = self.sems[key]
        self.prog[q].append((lambda eng=eng, sem=sem, out=out, in_=in_, kw=kw:
                             eng.dma_start(out=out, in_=in_, **kw).then_inc(sem, 16)))
        self.hist[key][v] = dict(self.known[q])
        for d in reads:
            d.r[key] = v
        for d in writes:
            d.w[key] = v

    def barrier(self):
        for e in self.engs:
            for k, v in self.count.items():
                if k == e or v == 0:
                    continue
                if self.known[e].get(k, 0) < v:
                    eng = self.engs[e]
                    sem = self.sems[k]
                    self.prog[e].append((lambda eng=eng, sem=sem, v=v: eng.wait_ge(sem, v)))
                    self.known[e][k] = v

    def finish(self):
        for q in ("sp", "pool", "act"):
            lst, idx = self.dma_pool[q]
            for key in lst:
                v = self.count[key]
                if v > 0 and self.known[q].get(key, 0) < v:
                    eng = self.engs[q]
                    sem = self.sems[key]
                    self.prog[q].append((lambda eng=eng, sem=sem, v=v: eng.wait_ge(sem, v)))
                    self.known[q][key] = v

    def emit(self, block):
        prog = self.prog

        @block.tensor
        def _(e):
            for f in prog["pe"]:
                f()

        @block.scalar
        def _(e):
            for f in prog["act"]:
                f()

        @block.vector
        def _(e):
            for f in prog["dve"]:
                f()

        @block.gpsimd
        def _(e):
            for f in prog["pool"]:
                f()

        @block.sync
        def _(e):
            for f in prog["sp"]:
                f()


from contextlib import ExitStack
from concourse.bass_utils import run_bass_kernel_spmd

T = 4096
NCTX = 256
NK = T + NCTX
NKT = NK // 128
D = 1024
ALPHA = 2.0 ** 0.25
EPS = 1e-6
LAMBDA_INIT = 0.2
NEG = -1.0e30


def build_program(skip=(), dbg=False, NCH=16):
    nc = bass.Bass("TRN2", target_bir_lowering=False)

    def din(name, shape, dt=F32):
        return nc.dram_tensor(name, shape, dt, kind="ExternalInput").ap()

    def dscr(name, shape, dt):
        kind = "ExternalOutput" if dbg else "Internal"
        return nc.dram_tensor(name, shape, dt, kind=kind).ap()

    x = din("x", [T, D]); ctx = din("ctx", [NCTX, D]); cT = din("cT", [128, 16])
    w_mod = din("w_mod", [D, 6144]); b_row = din("b_row", [1, 6144]); b_col = din("b_col", [128, 48])
    w_lat = din("w_lat", [D, 640]); w_da = din("w_da", [8, D, 640]); w_mla = din("w_mla", [8, 256, 512])
    w_gate = din("w_gate", [D, 2048]); w_pa = din("w_pa", [D, D]); w_pb = din("w_pb", [D, D]); w_out = din("w_out", [D, D])
    rows4 = din("rows4", [1, 4096]); lamrow = din("lamrow", [1, 256]); colsm = din("colsm", [128, 5])
    tabC = din("tabC", [128, NK]); tabS = din("tabS", [128, NK])
    ident_d = din("ident", [128, 128]); iota_d = din("iota", [128, 128])
    w_pq = din("w_pq", [D, 2048]); keysT = din("keysT", [128, 16 * 128])
    uT = din("uT", [64, 128, 2048]); vR = din("vR", [16384, D])
    out = nc.dram_tensor("out", [T, D], F32, kind="ExternalOutput").ap()

    yad = dscr("yad", [8, 128, T], BF16); ybd = dscr("ybd", [8, 128, T], BF16)
    x1d = dscr("x1d", [T, D], F32); h2Td = dscr("h2Td", [128, 8, T], BF16)
    uTb_r = nc.dram_tensor("uTb_r", [64, 128, 2048], BF16).ap(); vRb = nc.dram_tensor("vRb", [16384, D], BF16).ap()
    wpqb = nc.dram_tensor("wpqb", [D, 2048], BF16).ap()
    zTd = nc.dram_tensor("zTd", [128, 8, T], BF16).ap()
    bctd = nc.dram_tensor("bctd", [128, 8, 1024], F32).ap()

    with ExitStack() as gst:
        fw = FW(nc, gst)

        def sbt(st, name, shape, dt):
            return st.enter_context(nc.sbuf_tensor(name, shape, dt))

        PB_ = [gst.enter_context(nc.psum_tensor("bank%d" % i, [128, 512], F32)) for i in range(8)]
        dB = [Dep() for _ in range(8)]

        def mm(o, l, r, start, stop, reads, writes):
            fw.op("pe", lambda e: e.matmul(o, l, r, start=start, stop=stop), reads, writes)

        def tr(o, i, idn, reads, writes):
            fw.op("pe", lambda e: e.transpose(o, i, idn), reads, writes)

        def act(o, i, func, reads, writes, bias=0.0, scale=1.0, accum=None):
            if accum is None:
                fw.op("act", lambda e: e.activation(o, i, func, bias=bias, scale=scale), reads, writes)
            else:
                fw.op("act", lambda e: e.activation(o, i, func, bias=bias, scale=scale, accum_out=accum), reads, writes)

        def tt(eng, o, a, b, op, reads, writes):
            fw.op(eng, lambda e: e.tensor_tensor(o, a, b, op), reads, writes)

        def ts(eng, o, a, s1, s2, op0, op1, reads, writes):
            if s2 is None:
                fw.op(eng, lambda e: e.tensor_scalar(o, a, s1, None, op0), reads, writes)
            else:
                fw.op(eng, lambda e: e.tensor_scalar(o, a, s1, s2, op0, op1), reads, writes)

        def end_phase():
            fw.barrier()
            with nc.Block() as blk:
                fw.emit(blk)
            for k in fw.prog:
                fw.prog[k] = []

        def stt(eng, o, a, s, b, op0, op1, reads, writes):
            fw.op(eng, lambda e: e.scalar_tensor_tensor(o, a, s, b, op0, op1), reads, writes)

        def cp(eng, o, i, reads, writes):
            if eng == "act":
                fw.op("act", lambda e: e.copy(o, i), reads, writes)
            else:
                fw.op(eng, lambda e: e.tensor_copy(o, i), reads, writes)

        def recip(o, i, reads, writes):
            fw.op("dve", lambda e: e.reciprocal(o, i), reads, writes)

        def ld(o, i, writes, reads=()):
            fw.dma("sp", o, i, reads=reads, writes=writes)

        def st_(o, i, reads, writes=()):
            fw.dma("sp", o, i, reads=reads, writes=writes)

        IDENT = sbt(gst, "IDENT", [128, 128], F32); dIDENT = Dep()
        IOTA = sbt(gst, "IOTA", [128, 128], F32); dIOTA = Dep()
        ONES32 = sbt(gst, "ONES32", [128, 128], F32); dONES32 = Dep()
        ONES16 = sbt(gst, "ONES16", [128, 128], BF16); dONES16 = Dep()
        MODT = sbt(gst, "MODT", [128, 48, 2], F32); dMODT = Dep()
        SC1P = sbt(gst, "SC1P", [128, 8, 2], F32); dSC1P = Dep()
        NLAM = sbt(gst, "NLAM", [128, 1], F32); dNLAM = Dep()
        COLS = sbt(gst, "COLS", [128, 5], F32); dCOLS = Dep()
        SGC = sbt(gst, "SGC", [128, 1], F32); dSGC = Dep()
        ld(IDENT[:], ident_d, [dIDENT]); ld(IOTA[:], iota_d, [dIOTA]); ld(COLS[:], colsm, [dCOLS])
        fw.op("dve", lambda e: e.memset(ONES32[:], 1.0), (), [dONES32])
        fw.op("dve", lambda e: e.memset(ONES16[:], 1.0), (), [dONES16])
        fw.op("dve", lambda e: e.tensor_scalar(SGC[:], COLS[:, 4:5], 1.0 - LAMBDA_INIT, None, ALU.mult), [dCOLS], [dSGC])

        with ExitStack() as st:
            BCT = sbt(st, "BCT", [128, 8, 1024], F32); dBCT = Dep()
            CT = sbt(st, "CT", [128, 16], F32); dCT = Dep()
            SL = sbt(st, "SL", [128, 8, 2], F32); dSL = Dep()
            BCOL = sbt(st, "BCOL", [128, 48], F32); dBCOL = Dep()
            MODR = sbt(st, "MODR", [2, 6144], F32); dMODR = Dep()
            BROW = sbt(st, "BROW", [2, 6144], F32); dBROW = Dep()
            ROWS = sbt(st, "ROWS", [1, 4096], F32); dROWS = Dep()
            LAMR = sbt(st, "LAMR", [1, 256], F32); dLAMR = Dep()
            LAMB = sbt(st, "LAMB", [128, 256], F32); dLAMB = Dep()
            LTMP = sbt(st, "LTMP", [128, 4], F32); dLTMP = Dep()
            WM = [sbt(st, "WM%d" % i, [128, 8, 512], F32) for i in range(2)]; dWM = [Dep(), Dep()]
            ld(CT[:], cT, [dCT]); ld(BCOL[:], b_col, [dBCOL])
            ld(BROW[0:1, :], b_row, [dBROW]); ld(BROW[1:2, :], b_row, [dBROW])
            ld(ROWS[:], rows4, [dROWS]); ld(LAMR[:], lamrow, [dLAMR])
            act(SL[:, :, 0], CT[:, 0:8], ACTF.Silu, [dCT], [dSL])
            act(SL[:, :, 1], CT[:, 8:16], ACTF.Silu, [dCT], [dSL])
            wmv = w_mod.rearrange("(k p) n -> p k n", p=128)
            for j in range(12):
                W_ = WM[j % 2]; dW_ = dWM[j % 2]
                ld(W_[:], wmv[:, :, j * 512:(j + 1) * 512], [dW_])
                for cc in range(4):
                    for k in range(8):
                        mm(PB_[0][:, cc * 2:cc * 2 + 2], W_[:, k, cc * 128:(cc + 1) * 128], SL[:, k, :],
                           k == 0, k == 7, [dW_, dSL], [dB[0]])
                tt("dve", MODT[:, j * 4:(j + 1) * 4, :], PB_[0][:, 0:8].rearrange("p (c j) -> p c j", j=2),
                   BCOL[:, j * 4:(j + 1) * 4].unsqueeze(2).to_broadcast([128, 4, 2]), ALU.add, [dB[0], dBCOL], [dMODT])
                for k in range(8):
                    mm(PB_[1][0:2, :], SL[:, k, :], W_[:, k, :], k == 0, k == 7, [dW_, dSL], [dB[1]])
                tt("dve", MODR[:, j * 512:(j + 1) * 512], PB_[1][0:2, :], BROW[:, j * 512:(j + 1) * 512], ALU.add,
                   [dB[1], dBROW], [dMODR])
            ts("dve", SC1P[:], MODT[:, 8:16, :], 1.0, None, ALU.add, ALU.bypass, [dMODT], [dSC1P])
            srcs = [(MODR, 2048, dMODR), (MODR, 3072, dMODR), (MODR, 4096, dMODR), (MODR, 5120, dMODR),
                    (ROWS, 0, dROWS), (ROWS, 1024, dROWS), (ROWS, 2048, dROWS), (ROWS, 3072, dROWS)]
            for i, (src, off, dsrc) in enumerate(srcs):
                for hf in range(2):
                    bk = 2 + hf
                    mm(PB_[bk][:], ONES32[0:1, :], src[0:1, off + hf * 512: off + (hf + 1) * 512], True, True,
                       [dONES32, dsrc], [dB[bk]])
                    if i == 2:
                        ts("dve", BCT[:, i, hf * 512:(hf + 1) * 512], PB_[bk][:], 1.0, None, ALU.add, ALU.bypass,
                           [dB[bk]], [dBCT])
                    else:
                        cp("dve", BCT[:, i, hf * 512:(hf + 1) * 512], PB_[bk][:], [dB[bk]], [dBCT])
            st_(bctd, BCT[:], [dBCT])
            mm(PB_[4][:, 0:256], ONES32[0:1, :], LAMR[0:1, :], True, True, [dONES32, dLAMR], [dB[4]])
            cp("dve", LAMB[:], PB_[4][:, 0:256], [dB[4]], [dLAMB])
            tt("dve", LAMB[:, 0:64], LAMB[:, 0:64], LAMB[:, 64:128], ALU.mult, [dLAMB], [dLAMB])
            tt("dve", LAMB[:, 128:192], LAMB[:, 128:192], LAMB[:, 192:256], ALU.mult, [dLAMB], [dLAMB])
            fw.op("dve", lambda e: e.reduce_sum(LTMP[:, 0:1], LAMB[:, 0:64], AX.X), [dLAMB], [dLTMP])
            fw.op("dve", lambda e: e.reduce_sum(LTMP[:, 1:2], LAMB[:, 128:192], AX.X), [dLAMB], [dLTMP])
            act(LTMP[:, 2:4], LTMP[:, 0:2], ACTF.Exp, [dLTMP], [dLTMP])
            tt("dve", NLAM[:], LTMP[:, 3:4], LTMP[:, 2:3], ALU.subtract, [dLTMP], [dNLAM])
            ts("dve", NLAM[:], NLAM[:], -LAMBDA_INIT, None, ALU.add, ALU.bypass, [dNLAM], [dNLAM])
            end_phase()

        with ExitStack() as ast:
            HK = sbt(ast, "HK", [128, 8, NK], BF16); dHK = Dep()
            with ExitStack() as st:
                XT = [sbt(st, "XT%d" % i, [128, D], F32) for i in range(2)]; dXT = [Dep(), Dep()]
                for i in range(NKT):
                    X_ = XT[i % 2]; dX_ = dXT[i % 2]
                    src = ctx[i * 128:(i + 1) * 128, :] if i < 2 else x[(i - 2) * 128:(i - 1) * 128, :]
                    j = 1 if i < 2 else 0
                    ld(X_[:], src, [dX_])
                    for k in range(8):
                        bk = k // 4
                        tr(PB_[bk][:, (k % 4) * 128:(k % 4 + 1) * 128], X_[:, k * 128:(k + 1) * 128], IDENT[:],
                           [dX_, dIDENT], [dB[bk]])
                    for k in range(8):
                        bk = k // 4
                        ts("dve", HK[:, k, i * 128:(i + 1) * 128], PB_[bk][:, (k % 4) * 128:(k % 4 + 1) * 128],
                           SC1P[:, k, j:j + 1], MODT[:, k, j:j + 1], ALU.mult, ALU.add, [dB[bk], dSC1P, dMODT], [dHK])
                end_phase()

            with ExitStack() as rst:
                TCc = [sbt(rst, "TCc%d" % i, [128, 512], F32) for i in range(2)]; dTCc = [Dep(), Dep()]
                TSc = [sbt(rst, "TSc%d" % i, [128, 512], F32) for i in range(2)]; dTSc = [Dep(), Dep()]
                tabn = [0]

                def load_tab(np_, to, sl):
                    i = tabn[0] % 2; tabn[0] += 1
                    ld(TCc[i][0:np_, 0:sl], tabC[0:np_, to:to + sl], [dTCc[i]])
                    ld(TSc[i][0:np_, 0:sl], tabS[0:np_, to:to + sl], [dTSc[i]])
                    return TCc[i], dTCc[i], TSc[i], dTSc[i]
                QNT = sbt(rst, "QNT", [128, 2, T], BF16); dQNT = Dep()
                KVNT = sbt(rst, "KVNT", [128, 2, NK], BF16); dKVNT = Dep()
                KR = sbt(rst, "KR", [128, NK], BF16); dKR = Dep()
                fw.op("pool", lambda e: e.memset(KR[64:128, :], 0.0), (), [dKR])
                chunks_k = [(c * 512, 512) for c in range(8)] + [(4096, 256)]
                with ExitStack() as st:
                    WL32 = sbt(st, "WL32", [128, 8, 640], F32); dWL32 = Dep()
                    WL = sbt(st, "WL", [128, 8, 640], BF16); dWL = Dep()
                    SQ = sbt(st, "SQ", [128, 2, 512], F32); dSQ = Dep()
                    RS = sbt(st, "RS", [128, 512], F32); dRS = Dep()
                    T1 = sbt(st, "T1", [64, 512], F32); dT1 = Dep()
                    T2 = sbt(st, "T2", [64, 512], F32); dT2 = Dep()
                    ld(WL32[:], w_lat.rearrange("(k p) n -> p k n", p=128), [dWL32])
                    cp("act", WL[:], WL32[:], [dWL32], [dWL])

                    def latent(colbase, gcol, dst, ddst, koff, klen, dcol):
                        for m in range(2):
                            for k in range(8):
                                mm(PB_[m][:, 0:klen], WL[:, k, colbase + m * 128: colbase + (m + 1) * 128],
                                   HK[:, k, koff:koff + klen], k == 0, k == 7, [dWL, dHK], [dB[m]])
                            act(SQ[:, m, 0:klen], PB_[m][:, 0:klen], ACTF.Square, [dB[m]], [dSQ])
                        for m in range(2):
                            mm(PB_[2][:, 0:klen], ONES32[:], SQ[:, m, 0:klen], m == 0, m == 1, [dONES32, dSQ], [dB[2]])
                        act(RS[:, 0:klen], PB_[2][:, 0:klen], ACTF.Sqrt, [dB[2]], [dRS], bias=EPS, scale=1.0 / 256.0)
                        recip(RS[:, 0:klen], RS[:, 0:klen], [dRS], [dRS])
                        for m in range(2):
                            stt("dve", dst[:, m, dcol:dcol + klen], PB_[m][:, 0:klen], COLS[:, gcol + m:gcol + m + 1],
                                RS[:, 0:klen], ALU.mult, ALU.mult, [dB[m], dCOLS, dRS], [ddst])

                    for (ko, kl) in chunks_k:
                        latent(256, 2, KVNT, dKVNT, ko, kl, ko)
                        for k in range(8):
                            mm(PB_[3][0:64, 0:kl], WL[:, k, 512:576], HK[:, k, ko:ko + kl], k == 0, k == 7, [dWL, dHK], [dB[3]])
                        for k in range(8):
                            mm(PB_[4][0:64, 0:kl], WL[:, k, 576:640], HK[:, k, ko:ko + kl], k == 0, k == 7, [dWL, dHK], [dB[4]])
                        TC_, dTC_, TS_, dTS_ = load_tab(64, ko, kl)
                        tt("dve", T1[:, 0:kl], PB_[3][0:64, 0:kl], TC_[0:64, 0:kl], ALU.mult, [dB[3], dTC_], [dT1])
                        tt("dve", T2[:, 0:kl], PB_[4][0:64, 0:kl], TS_[0:64, 0:kl], ALU.mult, [dB[4], dTS_], [dT2])
                        tt("pool", KR[0:64, ko:ko + kl], T1[:, 0:kl], T2[:, 0:kl], ALU.add, [dT1, dT2], [dKR])
                    for c in range(8):
                        latent(0, 0, QNT, dQNT, NCTX + c * 512, 512, c * 512)
                    end_phase()
                with ExitStack() as st:
                    PT = [sbt(st, "PT%d" % i, [128, 512], BF16) for i in range(3)]; dPT = [Dep() for _ in range(3)]
                    RZ = sbt(st, "RZ", [128, 512], F32); dRZ = Dep()
                    ZA = sbt(st, "ZA", [128, 512], F32); dZA = Dep()
                    T1 = [sbt(st, "T1_%d" % i, [128, 512], F32) for i in range(1)]; dT1 = [Dep(), Dep()]
                    T2 = [sbt(st, "T2_%d" % i, [128, 512], F32) for i in range(1)]; dT2 = [Dep(), Dep()]
                    QH = [sbt(st, "QH%d" % i, [128, T], BF16) for i in range(1)]; dQH = [Dep(), Dep()]
                    KH = [sbt(st, "KH%d" % i, [128, NK], BF16) for i in range(1)]; dKH = [Dep(), Dep()]
                    VH = [sbt(st, "VH%d" % i, [128, NKT, 128], BF16) for i in range(1)]; dVH = [Dep(), Dep()]
                    AUX = sbt(st, "AUX", [128, NK], BF16); dAUX = Dep()
                    QRH = [AUX]; dQRH = [dAUX, dAUX]
                    WS = [sbt(st, "WS%d" % i, [128, 8, 128], F32) for i in range(1)]; dWS = [Dep(), Dep()]
                    WH = [sbt(st, "WH%d" % i, [128, 8, 640], BF16) for i in range(1)]; dWH = [Dep(), Dep()]
                    OM = [sbt(st, "OM%d" % i, [128, 512], F32) for i in range(2)]; dOM = [Dep(), Dep()]
                    DH = sbt(st, "DH", [128, 512], F32); dDH = Dep()
                    SQ2 = sbt(st, "SQ2", [128, 512], F32); dSQ2 = Dep()
                    RS2 = sbt(st, "RS2", [128, 512], F32); dRS2 = Dep()
                    YO = [sbt(st, "YO%d" % i, [128, 512], BF16) for i in range(2)]; dYO = [Dep(), Dep()]
                    tcount = [0]
                    SBK = [0, 1, 2, 7]

                    def attn(Sfn, Vt, dV, scale, si, consume, zdve=False):
                        PO = PB_[3 + 2 * si]; dPO = dB[3 + 2 * si]; PZ = PB_[4 + 2 * si]; dPZ = dB[4 + 2 * si]
                        Sfn(0)
                        Sfn(1)
                        for kt in range(NKT):
                            if kt + 2 < NKT:
                                Sfn(kt + 2)
                            b = kt % 3
                            act(PT[b][:], PB_[SBK[kt % 4]][:], ACTF.Exp, [dB[SBK[kt % 4]]], [dPT[b]], scale=scale)
                            mm(PO[:], Vt[:, kt, :], PT[b][:], kt == 0, kt == NKT - 1, [dV, dPT[b]], [dPO])
                            if not zdve:
                                mm(PZ[:], ONES16[:], PT[b][:], kt == 0, kt == NKT - 1, [dONES16, dPT[b]], [dPZ])
                            elif kt == 0:
                                cp("dve", ZA[:], PT[b][:], [dPT[b]], [dZA])
                            else:
                                tt("dve", ZA[:], ZA[:], PT[b][:], ALU.add, [dZA, dPT[b]], [dZA])
                        if zdve:
                            mm(PZ[:], ONES32[:], ZA[:], True, True, [dONES32, dZA], [dPZ])
                        recip(RZ[:], PZ[:], [dPZ], [dRZ])
                        consume(PO, dPO)

                    def rope_proj(W_, dW_, ca, cb, src, dsrc, so, sl, to, dst, ddst, do, np_, dst2=None, ddst2=None):
                        nk_ = src.shape[1]
                        for k in range(nk_):
                            mm(PB_[0][0:np_, 0:sl], W_[:, k, ca:ca + np_], src[:, k, so:so + sl], k == 0, k == nk_ - 1,
                               [dW_, dsrc], [dB[0]])
                        for k in range(nk_):
                            mm(PB_[1][0:np_, 0:sl], W_[:, k, cb:cb + np_], src[:, k, so:so + sl], k == 0, k == nk_ - 1,
                               [dW_, dsrc], [dB[1]])
                        i = 0
                        TC_, dTC_, TS_, dTS_ = load_tab(np_, to, sl)
                        tt("dve", T1[i][0:np_, 0:sl], PB_[0][0:np_, 0:sl], TC_[0:np_, 0:sl], ALU.mult, [dB[0], dTC_], [dT1[i]])
                        tt("dve", T2[i][0:np_, 0:sl], PB_[1][0:np_, 0:sl], TS_[0:np_, 0:sl], ALU.mult, [dB[1], dTS_], [dT2[i]])
                        if dst2 is None:
                            tt("pool", dst[0:np_, do:do + sl], T1[i][0:np_, 0:sl], T2[i][0:np_, 0:sl], ALU.add, [dT1[i], dT2[i]], [ddst])
                        else:
                            tt("pool", dst[0:64, do:do + sl], T1[i][0:64, 0:sl], T2[i][0:64, 0:sl], ALU.add, [dT1[i], dT2[i]], [ddst])
                            tt("pool", dst2[64:128, do:do + sl], T1[i][64:128, 0:sl], T2[i][64:128, 0:sl], ALU.add, [dT1[i], dT2[i]], [ddst2])

                    def vproj(W_, dW_, c0, src, dsrc, Vt, dVt):
                        nk_ = src.shape[1]
                        for g0 in range(0, NKT, 4):
                            n = min(4, NKT - g0)
                            for j in range(n):
                                kt = g0 + j
                                for k in range(nk_):
                                    mm(PB_[7][:, j * 128:(j + 1) * 128], src[:, k, kt * 128:(kt + 1) * 128], W_[:, k, c0:c0 + 128],
                                       k == 0, k == nk_ - 1, [dsrc, dW_], [dB[7]])
                            cp("act", Vt[:, g0:g0 + n, :], PB_[7][:, 0:n * 128].rearrange("p (a b) -> p a b", b=128), [dB[7]], [dVt])

                    cast_jobs = []
                    for i in range(32):
                        cast_jobs.append((uTb_r[i * 2:(i + 1) * 2], uT[i * 2:(i + 1) * 2]))
                    for i in range(32):
                        cast_jobs.append((vRb[i * 512:(i + 1) * 512, :], vR[i * 512:(i + 1) * 512, :]))
                    for i in range(2):
                        cast_jobs.append((wpqb[i * 512:(i + 1) * 512, :], w_pq[i * 512:(i + 1) * 512, :]))

                    def cast_step(n):
                        for _ in range(n):
                            if cast_jobs:
                                o_, i_ = cast_jobs.pop(0)
                                fw.dma("pool", o_, i_)
                    fw.op("pool", lambda e: e.memset(KH[0][64:128, :], 0.0), (), [dKH[0]])
                    fw.op("pool", lambda e: e.memset(AUX[0:64, :], 0.0), (), [dAUX])
                    if "diff" not in skip:
                      for h in range(8):
                        hb = 0
                        cast_step(5)
                        wv = w_da[h].rearrange("(k p) n -> p k n", p=128)
                        for pc in range(5):
                            ld(WS[0][:], wv[:, :, pc * 128:(pc + 1) * 128], [dWS[0]])
                            cp("dve", WH[hb][:, :, pc * 128:(pc + 1) * 128], WS[0][:], [dWS[0]], [dWH[hb]])
                        for qc in range(8):
                            rope_proj(WH[hb], dWH[hb], 0, 128, HK, dHK, NCTX + qc * 512, 512, NCTX + qc * 512, QH[hb], dQH[hb], qc * 512, 128)
                        for (ko, kl) in chunks_k:
                            rope_proj(WH[hb], dWH[hb], 256, 384, HK, dHK, ko, kl, ko, KH[hb], dKH[hb], ko, 128, AUX, dAUX)
                        vproj(WH[hb], dWH[hb], 512, HK, dHK, VH[hb], dVH[hb])
                        for qc in range(8):
                            for m in range(2):
                                def Sfn(kt, m=m, qc=qc):
                                    Km = KH[hb] if m == 0 else AUX
                                    mm(PB_[SBK[kt % 4]][:], Km[:, kt * 128:(kt + 1) * 128],
                                       QH[hb][:, qc * 512:(qc + 1) * 512], True, True, [dKH[hb], dAUX, dQH[hb]], [dB[SBK[kt % 4]]])

                                def consume(PO, dPO, m=m):
                                    tt("dve", OM[m][:], PO[:], RZ[:], ALU.mult, [dPO, dRZ], [dOM[m]])
                                attn(Sfn, VH[hb], dVH[hb], 0.125, m, consume)
                            stt("dve", DH[:], OM[1][:], NLAM[:, 0:1], OM[0][:], ALU.mult, ALU.add,
                                [dOM[0], dOM[1], dNLAM], [dDH])
                            sl_ = slice(qc * 512, (qc + 1) * 512)
                            act(SQ2[:], DH[:], ACTF.Square, [dDH], [dSQ2])
                            mm(PB_[7][:], ONES32[:], SQ2[:], True, True, [dONES32, dSQ2], [dB[7]])
                            act(RS2[:], PB_[7][:], ACTF.Sqrt, [dB[7]], [dRS2], bias=EPS, scale=1.0 / 128.0)
                            recip(RS2[:], RS2[:], [dRS2], [dRS2])
                            stt("dve", YO[qc % 2][:], DH[:], SGC[:, 0:1], RS2[:], ALU.mult, ALU.mult, [dDH, dSGC, dRS2], [dYO[qc % 2]])
                            st_(yad[h, :, sl_], YO[qc % 2][:], [dYO[qc % 2]])

                    WM32 = [sbt(st, "WM32_%d" % i, [128, 2, 512], F32) for i in range(1)]; dWM32 = [Dep(), Dep()]
                    WMb = [sbt(st, "WMb%d" % i, [128, 2, 512], BF16) for i in range(1)]; dWMb = [Dep(), Dep()]
                    if "mla" not in skip:
                      for h in range(8):
                        hb = 0
                        cast_step(5)
                        ld(WM32[hb][:], w_mla[h].rearrange("(k p) n -> p k n", p=128), [dWM32[hb]])
                        cp("dve", WMb[hb][:], WM32[hb][:], [dWM32[hb]], [dWMb[hb]])
                        for qc in range(8):
                            for kk in range(2):
                                mm(PB_[2][:], WMb[hb][:, kk, 0:128], QNT[:, kk, qc * 512:(qc + 1) * 512], kk == 0, kk == 1,
                                   [dWMb[hb], dQNT], [dB[2]])
                            cp("act", QH[hb][:, qc * 512:(qc + 1) * 512], PB_[2][:], [dB[2]], [dQH[hb]])
                            rope_proj(WMb[hb], dWMb[hb], 128, 192, QNT, dQNT, qc * 512, 512, NCTX + qc * 512, QRH[hb], dQRH[hb], qc * 512, 64)
                        for (ko, kl) in chunks_k:
                            for kk in range(2):
                                mm(PB_[2][:, 0:kl], WMb[hb][:, kk, 256:384], KVNT[:, kk, ko:ko + kl], kk == 0, kk == 1,
                                   [dWMb[hb], dKVNT], [dB[2]])
                            cp("act", KH[hb][:, ko:ko + kl], PB_[2][:, 0:kl], [dB[2]], [dKH[hb]])
                        vproj(WMb[hb], dWMb[hb], 384, KVNT, dKVNT, VH[hb], dVH[hb])
                        for qc in range(8):
                            def Sfn(kt, qc=qc):
                                mm(PB_[SBK[kt % 4]][:], KH[hb][:, kt * 128:(kt + 1) * 128], QH[hb][:, qc * 512:(qc + 1) * 512],
                                   True, False, [dKH[hb], dQH[hb]], [dB[SBK[kt % 4]]])
                                mm(PB_[SBK[kt % 4]][:], KR[:, kt * 128:(kt + 1) * 128], QRH[hb][:, qc * 512:(qc + 1) * 512],
                                   False, True, [dKR, dQRH[hb]], [dB[SBK[kt % 4]]])

                            def consume(PO, dPO, qc=qc):
                                tt("dve", YO[qc % 2][:], PO[:], RZ[:], ALU.mult, [dPO, dRZ], [dYO[qc % 2]])
                                st_(ybd[h, :, qc * 512:(qc + 1) * 512], YO[qc % 2][:], [dYO[qc % 2]])
                            attn(Sfn, VH[hb], dVH[hb], 192.0 ** -0.5, qc % 2, consume, zdve=True)
                    cast_step(100)
                    end_phase()
            with ExitStack() as st:
                WST = [sbt(st, "WST%d" % i, [128, 8, 256], F32) for i in range(2)]; dWST = [Dep(), Dep()]
                WPA = sbt(st, "WPA", [128, 8, D], BF16); dWPA = Dep()
                WPB = sbt(st, "WPB", [128, 8, D], BF16); dWPB = Dep()
                WG = sbt(st, "WG", [128, 8, 2048], BF16); dWG = Dep()
                YA = [sbt(st, "YA%d" % i, [128, 8, 512], BF16) for i in range(2)]; dYA = [Dep(), Dep()]
                YB = [sbt(st, "YB%d" % i, [128, 8, 512], BF16) for i in range(2)]; dYB = [Dep(), Dep()]
                GA = sbt(st, "GA", [128, 512], F32); dGA = Dep()
                GB = sbt(st, "GB", [128, 512], F32); dGB = Dep()
                U1 = sbt(st, "U1", [128, 512], F32); dU1 = Dep()
                U2 = sbt(st, "U2", [128, 512], F32); dU2 = Dep()
                ZT = [sbt(st, "ZT%d" % i, [128, 8, 512], BF16) for i in range(2)]; dZT = [Dep(), Dep()]
                wcnt = [0]

                def load_w(dst, ddst, src, ncols):
                    sv = src.rearrange("(k p) n -> p k n", p=128)
                    for c0 in range(0, ncols, 256):
                        i = wcnt[0] % 2; wcnt[0] += 1
                        ld(WST[i][:], sv[:, :, c0:c0 + 256], [dWST[i]])
                        cp("dve" if (wcnt[0] % 2) else "act", dst[:, :, c0:c0 + 256], WST[i][:], [dWST[i]], [ddst])
                load_w(WPA, dWPA, w_pa, D); load_w(WPB, dWPB, w_pb, D); load_w(WG, dWG, w_gate, 2048)
                for qc in range(8):
                    qb = qc % 2
                    sl_ = slice(qc * 512, (qc + 1) * 512)
                    ld(YA[qb][:], yad[:, :, sl_].rearrange("h p t -> p h t"), [dYA[qb]])
                    ld(YB[qb][:], ybd[:, :, sl_].rearrange("h p t -> p h t"), [dYB[qb]])
                    for oc in range(8):
                        oc_ = slice(oc * 128, (oc + 1) * 128)
                        for hh in range(8):
                            mm(PB_[0][:], WPA[:, hh, oc_], YA[qb][:, hh, :], hh == 0, hh == 7, [dWPA, dYA[qb]], [dB[0]])
                        for hh in range(8):
                            mm(PB_[1][:], WPB[:, hh, oc_], YB[qb][:, hh, :], hh == 0, hh == 7, [dWPB, dYB[qb]], [dB[1]])
                        for k in range(8):
                            mm(PB_[2][:], WG[:, k, oc * 128:(oc + 1) * 128], HK[:, k, NCTX + qc * 512:NCTX + (qc + 1) * 512],
                               k == 0, k == 7, [dWG, dHK], [dB[2]])
                        for k in range(8):
                            mm(PB_[3][:], WG[:, k, 1024 + oc * 128:1024 + (oc + 1) * 128], HK[:, k, NCTX + qc * 512:NCTX + (qc + 1) * 512],
                               k == 0, k == 7, [dWG, dHK], [dB[3]])
                        act(GA[:], PB_[2][:], ACTF.Sigmoid, [dB[2]], [dGA])
                        act(GB[:], PB_[3][:], ACTF.Sigmoid, [dB[3]], [dGB])
                        tt("dve", U1[:], PB_[0][:], GA[:], ALU.mult, [dB[0], dGA], [dU1])
                        tt("dve", U2[:], PB_[1][:], GB[:], ALU.mult, [dB[1], dGB], [dU2])
                        tt("pool", ZT[qb][:, oc, :], U1[:], U2[:], ALU.add, [dU1, dU2], [dZT[qb]])
                    st_(zTd[:, :, sl_], ZT[qb][:], [dZT[qb]])
                end_phase()
        with ExitStack() as st:
            WST = [sbt(st, "WSTb%d" % i, [128, 8, 256], F32) for i in range(2)]; dWST = [Dep(), Dep()]
            WOUT = sbt(st, "WOUT", [128, 8, D], BF16); dWOUT = Dep()
            BC = sbt(st, "BC5", [128, 5, D], F32); dBC = Dep()
            ZC = [sbt(st, "ZC%d" % i, [128, 8, 512], BF16) for i in range(2)]; dZC = [Dep(), Dep()]
            XR = [sbt(st, "XR%d" % i, [128, D], F32) for i in range(2)]; dXR = [Dep(), Dep()]
            TMP = sbt(st, "TMP", [128, D], F32); dTMP = Dep()
            PRE = sbt(st, "PRE", [128, D], F32); dPRE = Dep()
            JNK = sbt(st, "JNK", [128, D], F32); dJNK = Dep()
            X1 = [sbt(st, "X1_%d" % i, [128, D], F32) for i in range(2)]; dX1 = [Dep(), Dep()]
            H2 = sbt(st, "H2", [128, D], F32); dH2 = Dep()
            H2T = [sbt(st, "H2T%d" % i, [128, 8, 128], BF16) for i in range(2)]; dH2T = [Dep(), Dep()]
            STA = sbt(st, "STA", [128, 8], F32); dSTA = Dep()
            sv = w_out.rearrange("(k p) n -> p k n", p=128)
            for c0 in range(0, D, 256):
                i = (c0 // 256) % 2
                ld(WST[i][:], sv[:, :, c0:c0 + 256], [dWST[i]])
                cp("dve", WOUT[:, :, c0:c0 + 256], WST[i][:], [dWST[i]], [dWOUT])
            for i, j in enumerate([0, 1, 2, 4, 5]):
                ld(BC[:, i, :], bctd[:, j, :], [dBC])

            def layer_norm(PREt, dPREt, gi, bi, OUTt, dOUTt, BCt, dBCt):
                act(JNK[:], PREt[:], ACTF.Identity, [dPREt], [dJNK, dSTA], accum=STA[:, 0:1])
                act(JNK[:], PREt[:], ACTF.Square, [dPREt], [dJNK, dSTA], accum=STA[:, 1:2])
                ts("dve", STA[:, 2:3], STA[:, 0:1], 1.0 / D, None, ALU.mult, None, [dSTA], [dSTA])
                tt("dve", STA[:, 3:4], STA[:, 2:3], STA[:, 2:3], ALU.mult, [dSTA], [dSTA])
                stt("dve", STA[:, 4:5], STA[:, 1:2], 1.0 / D, STA[:, 3:4], ALU.mult, ALU.subtract, [dSTA], [dSTA])
                act(STA[:, 5:6], STA[:, 4:5], ACTF.Sqrt, [dSTA], [dSTA], bias=EPS, scale=1.0)
                recip(STA[:, 6:7], STA[:, 5:6], [dSTA], [dSTA])
                ts("dve", OUTt[:], PREt[:], STA[:, 2:3], STA[:, 6:7], ALU.subtract, ALU.mult, [dPREt, dSTA], [dOUTt])
                tt("pool", OUTt[:], OUTt[:], BCt[:, gi, :], ALU.mult, [dOUTt, dBCt], [dOUTt])
                tt("pool", OUTt[:], OUTt[:], BCt[:, bi, :], ALU.add, [dOUTt, dBCt], [dOUTt])

            for qc in range(8):
                qb = qc % 2
                ld(ZC[qb][:], zTd[:, :, qc * 512:(qc + 1) * 512], [dZC[qb]])
                for t4 in range(4):
                    ti = qc * 4 + t4
                    xb = ti % 2
                    ld(XR[xb][:], x[ti * 128:(ti + 1) * 128, :], [dXR[xb]])
                    for hf in range(2):
                        for oc in range(8):
                            mm(PB_[hf][:], ZC[qb][:, oc, t4 * 128:(t4 + 1) * 128], WOUT[:, oc, hf * 512:(hf + 1) * 512],
                               oc == 0, oc == 7, [dZC[qb], dWOUT], [dB[hf]])
                        tt("dve", TMP[:, hf * 512:(hf + 1) * 512], PB_[hf][:], BC[:, 0, hf * 512:(hf + 1) * 512], ALU.mult,
                           [dB[hf], dBC], [dTMP])
                    stt("dve", PRE[:], XR[xb][:], ALPHA, TMP[:], ALU.mult, ALU.add, [dXR[xb], dTMP], [dPRE])
                    layer_norm(PRE, dPRE, 3, 4, X1[xb], dX1[xb], BC, dBC)
                    st_(x1d[ti * 128:(ti + 1) * 128, :], X1[xb][:], [dX1[xb]])
                    tt("dve", H2[:], X1[xb][:], BC[:, 2, :], ALU.mult, [dX1[xb], dBC], [dH2])
                    tt("pool", H2[:], H2[:], BC[:, 1, :], ALU.add, [dH2, dBC], [dH2])
                    for k in range(8):
                        bk = 2 + k // 4
                        tr(PB_[bk][:, (k % 4) * 128:(k % 4 + 1) * 128], H2[:, k * 128:(k + 1) * 128], IDENT[:], [dH2, dIDENT], [dB[bk]])
                    for g in range(2):
                        cp("act", H2T[xb][:, g * 4:(g + 1) * 4, :], PB_[2 + g][:].rearrange("p (a b) -> p a b", b=128), [dB[2 + g]], [dH2T[xb]])
                    st_(h2Td[:, :, ti * 128:(ti + 1) * 128], H2T[xb][:], [dH2T[xb]])
            end_phase()
        with ExitStack() as st:
            GG = sbt(st, "GG", [128, 128, 256], BF16); dGG = Dep()
            KEYSb = sbt(st, "KEYSb", [128, 16, 128], BF16); dKEYS = Dep()
            BC7 = sbt(st, "BC7", [128, 3, D], F32); dBC7 = Dep()
            H2C = [sbt(st, "H2C%d" % i, [128, 8, 256], BF16) for i in range(2)]; dH2C = [Dep(), Dep()]
            WQ = [sbt(st, "WQ%d" % i, [128, 8, 128], BF16) for i in range(2)]; dWQ = [Dep(), Dep()]
            QPT = sbt(st, "QPT", [128, 16, 256], BF16); dQPT = Dep()
            SCB = [sbt(st, "SCB%d" % i, [128, 2048], F32) for i in range(2)]; dSCB = [Dep(), Dep()]
            BUFA = sbt(st, "BUFA", [128, 2048], F32); dBUFA = Dep()
            BUFB = sbt(st, "BUFB", [128, 2048], F32); dBUFB = Dep()
            V16 = sbt(st, "V16", [128, 16, 16], F32); dV16 = Dep()
            IX = sbt(st, "IX", [128, 16, 16], U32); dIX = Dep()
            IXF = sbt(st, "IXF", [128, 16, 16], F32); dIXF = Dep()
            CV = sbt(st, "CV", [128, 8, 16], F32); dCV = Dep()
            CPi = sbt(st, "CPi", [128, 8, 16], U32); dCPi = Dep()
            PF = sbt(st, "PF", [128, 8, 16], F32); dPF = Dep()
            AF = sbt(st, "AF", [128, 8, 16], F32); dAF = Dep()
            BF = sbt(st, "BF", [128, 8, 16], F32); dBF = Dep()
            SLT = [sbt(st, "SLT%d" % i, [128, 3, 128], F32) for i in range(2)]; dSLT = [Dep(), Dep()]
            EG = sbt(st, "EG", [128, 8, 16], F32); dEG = Dep()
            SG = sbt(st, "SG", [128, 8], F32); dSG = Dep()
            TR3 = [sbt(st, "TR3_%d" % i, [128, 384], F32) for i in range(2)]; dTR3 = [Dep(), Dep()]
            OH1 = [sbt(st, "OH1_%d" % i, [128, 16, 128], BF16) for i in range(2)]; dOH1 = [Dep(), Dep()]
            OH2 = [sbt(st, "OH2_%d" % i, [128, 16, 128], BF16) for i in range(2)]; dOH2 = [Dep(), Dep()]
            NUB = 3
            UG = [sbt(st, "UG%d" % i, [128, 8, 256], BF16) for i in range(NUB)]; dUG = [Dep() for _ in range(NUB)]
            VG = [sbt(st, "VG%d" % i, [128, 2, D], BF16) for i in range(NUB)]; dVG = [Dep() for _ in range(NUB)]
            GE = [sbt(st, "GE%d" % i, [128, 256], BF16) for i in range(2)]; dGE = [Dep(), Dep()]
            WT = [sbt(st, "WT%d" % i, [128, 256], BF16) for i in range(2)]; dWT = [Dep(), Dep()]
            TMP = sbt(st, "TMP7", [128, D], F32); dTMP = Dep()
            OUTt = sbt(st, "OUTt", [128, D], F32); dOUTt = Dep()
            X1t = OUTt; dX1t = dOUTt
            STA = sbt(st, "STA7", [128, 8], F32); dSTA = Dep()
            dB4h = [Dep(), Dep()]
            THR16 = sbt(st, "THR16", [128, 16], F32); dTHR16 = Dep()
            ts("dve", THR16[:], IOTA[:, 0:16], 16.0, None, ALU.mult, None, [dIOTA], [dTHR16])
            fw.dma("pool", KEYSb[:].rearrange("p a b -> p (a b)"), keysT, writes=[dKEYS])
            for i, j in enumerate([3, 6, 7]):
                ld(BC7[:, i, :], bctd[:, j, :], [dBC7])
            wqv = wpqb.rearrange("(k p) n -> p k n", p=128)
            WKv = BUFB[:].rearrange("p (a b) -> p a b", b=128)
            OHA = BUFB[:].rearrange("p (h a b) -> p h a b", a=16, b=16); dOHA = dBUFB
            CAND = BUFA[:].rearrange("p (h a b) -> p h a b", a=16, b=16)
            CANDf = BUFA[:].rearrange("p (h c) -> p h c", c=256)
            CWf = BUFB[:].rearrange("p (h c) -> p h c", c=256)
            V16v = V16[:].rearrange("p (h m) a -> p h m a", m=2)
            IXFv = IXF[:].rearrange("p (h m) a -> p h m a", m=2)
            B16 = [128, 8, 16, 16]
            ohc = [0]; gcn = [0]; ugn = [0]

            def ln7(PREt, dPREt, OUTt_, dOUTt_):
                act(OUTt_[:], PREt[:], ACTF.Identity, [dPREt], [dOUTt_, dSTA], accum=STA[:, 0:1])
                act(OUTt_[:], PREt[:], ACTF.Square, [dPREt], [dOUTt_, dSTA], accum=STA[:, 1:2])
                ts("dve", STA[:, 2:3], STA[:, 0:1], 1.0 / D, None, ALU.mult, None, [dSTA], [dSTA])
                tt("dve", STA[:, 3:4], STA[:, 2:3], STA[:, 2:3], ALU.mult, [dSTA], [dSTA])
                stt("dve", STA[:, 4:5], STA[:, 1:2], 1.0 / D, STA[:, 3:4], ALU.mult, ALU.subtract, [dSTA], [dSTA])
                act(STA[:, 5:6], STA[:, 4:5], ACTF.Sqrt, [dSTA], [dSTA], bias=EPS, scale=1.0)
                recip(STA[:, 6:7], STA[:, 5:6], [dSTA], [dSTA])
                ts("dve", OUTt_[:], PREt[:], STA[:, 2:3], STA[:, 6:7], ALU.subtract, ALU.mult, [dPREt, dSTA], [dOUTt_])
                tt("pool", OUTt_[:], OUTt_[:], BC7[:, 1, :], ALU.mult, [dOUTt_, dBC7], [dOUTt_])
                tt("pool", OUTt_[:], OUTt_[:], BC7[:, 2, :], ALU.add, [dOUTt_, dBC7], [dOUTt_])

            def top16_batch(n, vals, idxs, src, wk, dvals, didx, dsrc, dwk):
                for g in range(n):
                    fw.op("dve", lambda e, g=g: e.max(out=vals(g)[:, 0:8], in_=src(g)), [dsrc], [dvals])
                for g in range(n):
                    fw.op("dve", lambda e, g=g: e.max_index(out=idxs(g)[:, 0:8], in_max=vals(g)[:, 0:8], in_values=src(g)), [dsrc, dvals], [didx])
                for g in range(n):
                    fw.op("dve", lambda e, g=g: e.match_replace(out=wk(g), in_to_replace=vals(g)[:, 0:8], in_values=src(g), imm_value=NEG),
                          [dsrc, dvals], [dwk])
                for g in range(n):
                    fw.op("dve", lambda e, g=g: e.max(out=vals(g)[:, 8:16], in_=wk(g)), [dwk], [dvals])
                for g in range(n):
                    fw.op("dve", lambda e, g=g: e.max_index(out=idxs(g)[:, 8:16], in_max=vals(g)[:, 8:16], in_values=wk(g)), [dwk, dvals], [didx])

            def stageA_front(c):
                cb = c % 2
                ld(H2C[cb][:], h2Td[:, :, c * 256:(c + 1) * 256], [dH2C[cb]])
                for hm in range(16):
                    ld(WQ[hm % 2][:], wqv[:, :, hm * 128:(hm + 1) * 128], [dWQ[hm % 2]])
                    r = (hm % 2) * 256
                    for k in range(8):
                        mm(PB_[6][:, r:r + 256], WQ[hm % 2][:, k, :], H2C[cb][:, k, :], k == 0, k == 7, [dWQ[hm % 2], dH2C[cb]], [dB[6]])
                    if hm % 2 == 1:
                        cp("act", QPT[:, hm - 1:hm + 1, :], PB_[6][:].rearrange("p (a b) -> p a b", b=256), [dB[6]], [dQPT])
                for t2 in range(2):
                    SC = SCB[t2][:].rearrange("p (a b) -> p a b", b=128)
                    for g in range(4):
                        for j in range(4):
                            hm = g * 4 + j
                            mm(PB_[7][:, j * 128:(j + 1) * 128], QPT[:, hm, t2 * 128:(t2 + 1) * 128], KEYSb[:, hm, :], True, True,
                               [dQPT, dKEYS], [dB[7]])
                        cp("act", SCB[t2][:, g * 512:(g + 1) * 512], PB_[7][:], [dB[7]], [dSCB[t2]])
                for t2 in range(2):
                    SC = SCB[t2][:].rearrange("p (a b) -> p a b", b=128)
                    top16_batch(16, lambda g: V16[:, g, :], lambda g: IX[:, g, :], lambda g, SC=SC: SC[:, g, :], lambda g: WKv[:, g, :],
                                dV16, dIX, dSCB[t2], dBUFB)
                    cp("dve", IXF[:], IX[:], [dIX], [dIXF])
                    tt("dve", CAND, V16v[:, :, 0, :].unsqueeze(3).to_broadcast(B16), V16v[:, :, 1, :].unsqueeze(2).to_broadcast(B16),
                       ALU.add, [dV16], [dBUFA])
                    top16_batch(8, lambda g: CV[:, g, :], lambda g: CPi[:, g, :], lambda g: CANDf[:, g, :], lambda g: CWf[:, g, :],
                                dCV, dCPi, dBUFA, dBUFB)
                    cp("dve", PF[:], CPi[:], [dCPi], [dPF])
                    tt("dve", OHA, PF[:].unsqueeze(3).to_broadcast(B16), THR16[:, 0:16].unsqueeze(1).unsqueeze(1).to_broadcast(B16),
                       ALU.is_ge, [dPF, dTHR16], [dOHA])
                    fw.op("dve", lambda e: e.tensor_reduce(AF[:], OHA, AX.X, ALU.add), [dOHA], [dAF])
                    ts("dve", AF[:], AF[:], -1.0, None, ALU.add, None, [dAF], [dAF])
                    stt("dve", BF[:], AF[:], -16.0, PF[:], ALU.mult, ALU.add, [dAF, dPF], [dBF])
                    iob = IOTA[:, 0:16].unsqueeze(1).unsqueeze(1).to_broadcast(B16)
                    for (XF, dXF, mi) in ((AF, dAF, 0), (BF, dBF, 1)):
                        tt("dve", OHA, XF[:].unsqueeze(3).to_broadcast(B16), iob, ALU.is_equal, [dXF, dIOTA], [dOHA])
                        tt("dve", OHA, OHA, IXFv[:, :, mi, :].unsqueeze(2).to_broadcast(B16), ALU.mult, [dOHA, dIXF], [dOHA])
                        fw.op("dve", lambda e, mi=mi, t2=t2: e.tensor_reduce(SLT[t2][:, mi, :].rearrange("p (a b) -> p a b", b=16), OHA, AX.X, ALU.add),
                              [dOHA], [dSLT[t2]])
                    tt("dve", EG[:], CV[:], CV[:, :, 0:1].to_broadcast([128, 8, 16]), ALU.subtract, [dCV], [dEG])
                    act(EG[:], EG[:], ACTF.Exp, [dEG], [dEG])
                    fw.op("dve", lambda e: e.reduce_sum(SG[:], EG[:], AX.X), [dEG], [dSG])
                    recip(SG[:], SG[:], [dSG], [dSG])
                    tt("dve", SLT[t2][:, 2, :].rearrange("p (a b) -> p a b", b=16), EG[:], SG[:].unsqueeze(2).to_broadcast([128, 8, 16]), ALU.mult,
                       [dEG, dSG], [dSLT[t2]])

            def stageA_back(c):
                for t2 in range(2):
                    for i in range(3):
                        tr(PB_[6][:, i * 128:(i + 1) * 128], SLT[t2][:, i, :], IDENT[:], [dSLT[t2], dIDENT], [dB[6]])
                    cp("act", TR3[t2][:], PB_[6][:, 0:384], [dB[6]], [dTR3[t2]])

            def stageB(c):
                for t2 in range(2):
                    T3 = TR3[t2]; dT3 = dTR3[t2]
                    for s in range(8):
                        ob = ohc[0] % 2; ohc[0] += 1
                        t0 = s * 16
                        B3 = [128, 16, 128]
                        iot = IOTA[:].unsqueeze(1).to_broadcast(B3)
                        tt("dve", OH1[ob][:], iot, T3[:, t0:t0 + 16].unsqueeze(2).to_broadcast(B3), ALU.is_equal,
                           [dIOTA, dT3], [dOH1[ob]])
                        tt("dve", OH1[ob][:], OH1[ob][:], T3[:, 256 + t0:256 + t0 + 16].unsqueeze(2).to_broadcast(B3), ALU.mult,
                           [dOH1[ob], dT3], [dOH1[ob]])
                        tt("dve", OH2[ob][:], iot, T3[:, 128 + t0:128 + t0 + 16].unsqueeze(2).to_broadcast(B3), ALU.is_equal,
                           [dIOTA, dT3], [dOH2[ob]])
                        for q4 in range(4):
                            gb = 5 if (gcn[0] % 2 == 0) else 7
                            gcn[0] += 1
                            for j in range(4):
                                tl = q4 * 4 + j
                                mm(PB_[gb][:, j * 128:(j + 1) * 128], OH1[ob][:, tl, :], OH2[ob][:, tl, :], True, True,
                                   [dOH1[ob], dOH2[ob]], [dB[gb]])
                            tg = t2 * 128 + t0 + q4 * 4
                            cp("act", GG[:, :, tg:tg + 4].rearrange("p i t -> p t i"),
                               PB_[gb][:].rearrange("p (t i) -> p t i", i=128), [dB[gb]], [dGG])

            def dense(c):
                cb = c % 2

                def stage1(i2):
                    gi = i2 // 2; bl = i2 % 2
                    if bl == 0:
                        ub = ugn[0] % NUB; ugn[0] += 1
                        ld(UG[ub][:].rearrange("p k n -> p (k n)"), uTb_r[gi], [dUG[ub]])
                        ld(VG[ub][:], vRb[gi * 256:(gi + 1) * 256, :].rearrange("(b p) n -> p b n", p=128), [dVG[ub]])
                    ub = (ugn[0] - 1) % NUB
                    r = (i2 % 2) * 256
                    for k in range(8):
                        mm(PB_[4][:, r:r + 256], UG[ub][:, k, bl * 128:(bl + 1) * 128], H2C[cb][:, k, :], k == 0, k == 7,
                           [dUG[ub], dH2C[cb]], [dB4h[i2 % 2]])
                    return ub
                ubs = {}
                ubs[0] = stage1(0)
                for i2 in range(128):
                    if i2 + 1 < 128:
                        ubs[i2 + 1] = stage1(i2 + 1)
                    bl = i2 % 2; ub = ubs[i2]
                    r = (i2 % 2) * 256
                    act(GE[i2 % 2][:], PB_[4][:, r:r + 256], ACTF.Gelu_apprx_tanh, [dB4h[i2 % 2]], [dGE[i2 % 2]])
                    tt("pool", WT[i2 % 2][:], GE[i2 % 2][:], GG[:, i2, :], ALU.mult, [dGE[i2 % 2], dGG], [dWT[i2 % 2]])
                    for t2 in range(2):
                        for hf in range(2):
                            bk = t2 * 2 + hf
                            mm(PB_[bk][:], WT[i2 % 2][:, t2 * 128:(t2 + 1) * 128], VG[ub][:, bl, hf * 512:(hf + 1) * 512],
                               i2 == 0, i2 == 127, [dWT[i2 % 2], dVG[ub]], [dB[bk]])

            def epilogue(c):
                for t2 in range(2):
                    ti = c * 2 + t2
                    ld(X1t[:], x1d[ti * 128:(ti + 1) * 128, :], [dX1t])
                    for hf in range(2):
                        tt("dve", TMP[:, hf * 512:(hf + 1) * 512], PB_[t2 * 2 + hf][:], BC7[:, 0, hf * 512:(hf + 1) * 512], ALU.mult,
                           [dB[t2 * 2 + hf], dBC7], [dTMP])
                    stt("dve", TMP[:], X1t[:], ALPHA, TMP[:], ALU.mult, ALU.add, [dX1t, dTMP], [dTMP])
                    ln7(TMP, dTMP, OUTt, dOUTt)
                    st_(out[ti * 128:(ti + 1) * 128, :], OUTt[:], [dOUTt])

            stageA_front(0); stageA_back(0); stageB(0)
            for c in range(NCH):
                if c + 1 < NCH:
                    stageA_front(c + 1)
                dense(c)
                epilogue(c)
                if c + 1 < NCH:
                    stageA_back(c + 1)
                    stageB(c + 1)
            fw.finish()
            end_phase()
    return nc


def _prep_shared(inp):
    f = np.float32
    w_in = np.asarray(inp["w_in"][0], f)
    perm64 = np.concatenate([np.arange(32, 64), np.arange(0, 32)])
    perm128 = np.concatenate([perm64, 64 + perm64])
    Wq = w_in[:, 0:1024]; Wk = w_in[:, 3328:4352]; Wv = w_in[:, 4352:5376]
    w_da = np.stack([np.concatenate([Wq[:, h * 128:(h + 1) * 128], Wq[:, h * 128 + perm128],
                                     Wk[:, h * 128:(h + 1) * 128], Wk[:, h * 128 + perm128],
                                     Wv[:, h * 128:(h + 1) * 128]], axis=1) for h in range(8)])
    w_lat = np.concatenate([w_in[:, 1024:1280], w_in[:, 5376:5632], w_in[:, 5632:5696], w_in[:, 5632 + perm64]], axis=1)
    wq = np.asarray(inp["w_q_up"][0], f); wkv = np.asarray(inp["w_kv_up"][0], f)
    w_mla = np.stack([np.concatenate([wq[:, h * 192:h * 192 + 128], wq[:, h * 192 + 128:h * 192 + 192],
                                      wq[:, h * 192 + 128 + perm64], wkv[:, h * 256:h * 256 + 128],
                                      wkv[:, h * 256 + 128:h * 256 + 256]], axis=1) for h in range(8)])
    rows4 = np.concatenate([inp["ln1_g"][0], inp["ln1_b"][0], inp["ln2_g"][0], inp["ln2_b"][0]]).astype(f)[None]
    lamrow = np.concatenate([inp["lambda_q1"][0], inp["lambda_k1"][0], inp["lambda_q2"][0], inp["lambda_k2"][0]]).astype(f)[None]
    colsm = np.concatenate([np.asarray(inp["q_norm_g"][0], f).reshape(2, 128).T, np.asarray(inp["kv_norm_g"][0], f).reshape(2, 128).T,
                            np.asarray(inp["subln_g"][0], f).reshape(128, 1)], axis=1)
    quarter = 16
    inv = (10000.0 ** (-np.arange(quarter, dtype=f) / quarter)).astype(f)
    tpos = np.arange(T)
    ang = np.concatenate([(tpos // 64).astype(f)[:, None] * inv, (tpos % 64).astype(f)[:, None] * inv], axis=1).astype(f)
    p = np.arange(128)
    cosT = np.cos(ang).astype(f).T[p % 32]
    sinT = np.sin(ang).astype(f).T[p % 32]
    sign = np.where((p % 64) < 32, -1.0, 1.0).astype(f)[:, None]
    tabC = np.concatenate([np.ones((128, NCTX), f), cosT], axis=1)
    tabS = np.concatenate([np.zeros((128, NCTX), f), sinT * sign], axis=1)
    b_mod = np.asarray(inp["b_mod"][0], f)
    keys = np.asarray(inp["peer_keys"][0], f)
    keysT = np.ascontiguousarray(keys.transpose(3, 0, 1, 2).reshape(128, 16 * 128))
    U = np.asarray(inp["peer_u"][0], f); V = np.asarray(inp["peer_v"][0], f)
    uT0 = U.reshape(128, 128, 1024).transpose(2, 1, 0).reshape(8, 128, 64, 256)
    uT = np.ascontiguousarray(uT0.transpose(2, 1, 0, 3).reshape(64, 128, 2048))
    vR = np.ascontiguousarray(V.reshape(128, 128, 1024).transpose(1, 0, 2).reshape(16384, 1024))
    sh = {
        "w_mod": np.ascontiguousarray(inp["w_mod"][0], f), "b_row": b_mod[None].copy(),
        "b_col": np.ascontiguousarray(b_mod.reshape(48, 128).T),
        "w_lat": np.ascontiguousarray(w_lat), "w_da": np.ascontiguousarray(w_da), "w_mla": np.ascontiguousarray(w_mla),
        "w_gate": np.ascontiguousarray(w_in[:, 1280:3328]),
        "w_pa": np.ascontiguousarray(inp["w_pa"][0], f), "w_pb": np.ascontiguousarray(inp["w_pb"][0], f),
        "w_out": np.ascontiguousarray(inp["w_out"][0], f),
        "rows4": rows4, "lamrow": lamrow, "colsm": np.ascontiguousarray(colsm),
        "tabC": np.ascontiguousarray(tabC), "tabS": np.ascontiguousarray(tabS),
        "ident": np.eye(128, dtype=f), "iota": np.ascontiguousarray(np.tile(np.arange(128, dtype=f)[None], (128, 1))),
        "w_pq": np.ascontiguousarray(inp["w_pq"][0], f), "keysT": keysT, "uT": uT, "vR": vR,
    }
    return sh


def _in_maps(inp):
    sh = _prep_shared(inp)
    f = np.float32
    cctx = np.asarray(inp["c_ctx"], f).reshape(8, 128).T
    maps = []
    for b in range(8):
        m = dict(sh)
        m["x"] = np.ascontiguousarray(inp["x"][b], f)
        m["ctx"] = np.ascontiguousarray(inp["ctx"][b], f)
        m["cT"] = np.ascontiguousarray(np.concatenate([np.asarray(inp["c"][b], f).reshape(8, 128).T, cctx], axis=1))
        maps.append(m)
    return maps


def kernel(**inputs):
    inp = {k: np.asarray(v) for k, v in inputs.items()}
    nc = build_program()
    res = run_bass_kernel_spmd(nc, _in_maps(inp), core_ids=list(range(8)))
    return np.stack([np.asarray(r["out"], np.float32) for r in res.results], axis=0)
```

```python
import numpy as np
import concourse.bass as bass
import concourse.mybir as mybir

F32 = mybir.dt.float32
BF16 = mybir.dt.bfloat16
U32 = mybir.dt.uint32
ALU = mybir.AluOpType
ACTF = mybir.ActivationFunctionType
AX = mybir.AxisListType


class Dep:
    __slots__ = ("w", "r")

    def __init__(self):
        self.w = {}
        self.r = {}


class FW:
    NDMA = 24
    SELF_SYNC = True
    NOSELF = ("act",)

    def __init__(self, nc, stack):
        self.nc = nc
        self.engs = {"pe": nc.tensor, "act": nc.scalar, "dve": nc.vector,
                     "pool": nc.gpsimd, "sp": nc.sync}
        self.sems = {}
        self.count = {}
        self.known = {}
        self.hist = {}
        self.prog = {k: [] for k in self.engs}
        for k in self.engs:
            self.sems[k] = stack.enter_context(nc.semaphore("s_" + k))
            self.count[k] = 0
            self.known[k] = {}
            self.hist[k] = {}
        self.dma_pool = {}
        for q in ("sp", "pool", "act"):
            lst = []
            for i in range(self.NDMA):
                key = "d_%s_%d" % (q, i)
                self.sems[key] = stack.enter_context(nc.semaphore(key))
                self.count[key] = 0
                self.hist[key] = {}
                lst.append(key)
            self.dma_pool[q] = [lst, 0]

    def _learn(self, e, key, val):
        kn = self.known[e]
        if kn.get(key, 0) < val:
            kn[key] = val
        snap = self.hist[key].get(val)
        if snap:
            for k2, v2 in snap.items():
                if kn.get(k2, 0) < v2:
                    kn[k2] = v2

    def _waits(self, e, reads, writes, extra=()):
        need = {}
        for d in reads:
            for k, v in d.w.items():
                if need.get(k, 0) < v:
                    need[k] = v
        for d in writes:
            for k, v in d.w.items():
                if need.get(k, 0) < v:
                    need[k] = v
            for k, v in d.r.items():
                if need.get(k, 0) < v:
                    need[k] = v
        for k, v in extra:
            if need.get(k, 0) < v:
                need[k] = v
        kn = self.known[e]
        for k, v in need.items():
            if k == e and (e == "pe" or e in self.NOSELF):
                continue
            if kn.get(k, 0) >= v:
                continue
            eng = self.engs[e]
            sem = self.sems[k]
            self.prog[e].append((lambda eng=eng, sem=sem, v=v: eng.wait_ge(sem, v)))
            self._learn(e, k, v)

    def op(self, e, fn, reads=(), writes=()):
        self._waits(e, reads, writes)
        self.count[e] += 1
        c = self.count[e]
        eng = self.engs[e]
        sem = self.sems[e]
        self.prog[e].append((lambda eng=eng, sem=sem, fn=fn: fn(eng).then_inc(sem, 1)))
        self.hist[e][c] = dict(self.known[e])
        for d in reads:
            d.r[e] = c
        for d in writes:
            d.w[e] = c

    def dma(self, q, out, in_, reads=(), writes=(), **kw):
        lst, idx = self.dma_pool[q]
        key = lst[idx % len(lst)]
        self.dma_pool[q][1] = idx + 1
        prev = self.count[key]
        extra = [(key, prev)] if prev > 0 else []
        self._waits(q, reads, writes, extra)
        self.count[key] = prev + 16
        v = prev + 16
        eng = self.engs[q]
        sem = self.sems[key]
        self.prog[q].append((lambda eng=eng, sem=sem, out=out, in_=in_, kw=kw:
                             eng.dma_start(out=out, in_=in_, **kw).then_inc(sem, 16)))
        self.hist[key][v] = dict(self.known[q])
        for d in reads:
            d.r[key] = v
        for d in writes:
            d.w[key] = v

    def barrier(self):
        for e in self.engs:
            for k, v in self.count.items():
                if k == e or v == 0:
                    continue
                if self.known[e].get(k, 0) < v:
                    eng = self.engs[e]
                    sem = self.sems[k]
                    self.prog[e].append((lambda eng=eng, sem=sem, v=v: eng.wait_ge(sem, v)))
                    self.known[e][k] = v

    def finish(self):
        for q in ("sp", "pool", "act"):
            lst, idx = self.dma_pool[q]
            for key in lst:
                v = self.count[key]
                if v > 0 and self.known[q].get(key, 0) < v:
                    eng = self.engs[q]
                    sem = self.sems[key]
                    self.prog[q].append((lambda eng=eng, sem=sem, v=v: eng.wait_ge(sem, v)))
                    self.known[q][key] = v

    def emit(self, block):
        prog = self.prog

        @block.tensor
        def _(e):
            for f in prog["pe"]:
                f()

        @block.scalar
        def _(e):
            for f in prog["act"]:
                f()

        @block.vector
        def _(e):
            for f in prog["dve"]:
                f()

        @block.gpsimd
        def _(e):
            for f in prog["pool"]:
                f()

        @block.sync
        def _(e):
            for f in prog["sp"]:
                f()


from contextlib import ExitStack
from concourse.bass_utils import run_bass_kernel_spmd

T = 4096
NCTX = 256
NK = T + NCTX
NKT = NK // 128
D = 1024
ALPHA = 2.0 ** 0.25
EPS = 1e-6
LAMBDA_INIT = 0.2
NEG = -1.0e30


def build_program(skip=(), dbg=False, NCH=16):
    nc = bass.Bass("TRN2", target_bir_lowering=False)

    def din(name, shape, dt=F32):
        return nc.dram_tensor(name, shape, dt, kind="ExternalInput").ap()

    def dscr(name, shape, dt):
        kind = "ExternalOutput" if dbg else "Internal"
        return nc.dram_tensor(name, shape, dt, kind=kind).ap()

    x = din("x", [T, D]); ctx = din("ctx", [NCTX, D]); cT = din("cT", [128, 16])
    w_mod = din("w_mod", [D, 6144]); b_row = din("b_row", [1, 6144]); b_col = din("b_col", [128, 48])
    w_lat = din("w_lat", [D, 640]); w_da = din("w_da", [8, D, 640]); w_mla = din("w_mla", [8, 256, 512])
    w_gate = din("w_gate", [D, 2048]); w_pa = din("w_pa", [D, D]); w_pb = din("w_pb", [D, D]); w_out = din("w_out", [D, D])
    rows4 = din("rows4", [1, 4096]); lamrow = din("lamrow", [1, 256]); colsm = din("colsm", [128, 5])
    tabC = din("tabC", [128, NK]); tabS = din("tabS", [128, NK])
    ident_d = din("ident", [128, 128]); iota_d = din("iota", [128, 128])
    w_pq = din("w_pq", [D, 2048]); keysT = din("keysT", [128, 16 * 128])
    uT = din("uT", [64, 128, 2048]); vR = din("vR", [16384, D])
    out = nc.dram_tensor("out", [T, D], F32, kind="ExternalOutput").ap()

    yad = dscr("yad", [8, 128, T], BF16); ybd = dscr("ybd", [8, 128, T], BF16)
    x1d = dscr("x1d", [T, D], F32); h2Td = dscr("h2Td", [128, 8, T], BF16)
    uTb_r = nc.dram_tensor("uTb_r", [64, 128, 2048], BF16).ap(); vRb = nc.dram_tensor("vRb", [16384, D], BF16).ap()
    wpqb = nc.dram_tensor("wpqb", [D, 2048], BF16).ap()
    zTd = nc.dram_tensor("zTd", [128, 8, T], BF16).ap()
    bctd = nc.dram_tensor("bctd", [128, 8, 1024], F32).ap()

    with ExitStack() as gst:
        fw = FW(nc, gst)

        def sbt(st, name, shape, dt):
            return st.enter_context(nc.sbuf_tensor(name, shape, dt))

        PB_ = [gst.enter_context(nc.psum_tensor("bank%d" % i, [128, 512], F32)) for i in range(8)]
        dB = [Dep() for _ in range(8)]

        def mm(o, l, r, start, stop, reads, writes):
            fw.op("pe", lambda e: e.matmul(o, l, r, start=start, stop=stop), reads, writes)

        def tr(o, i, idn, reads, writes):
            fw.op("pe", lambda e: e.transpose(o, i, idn), reads, writes)

        def act(o, i, func, reads, writes, bias=0.0, scale=1.0, accum=None):
            if accum is None:
                fw.op("act", lambda e: e.activation(o, i, func, bias=bias, scale=scale), reads, writes)
            else:
                fw.op("act", lambda e: e.activation(o, i, func, bias=bias, scale=scale, accum_out=accum), reads, writes)

        def tt(eng, o, a, b, op, reads, writes):
            fw.op(eng, lambda e: e.tensor_tensor(o, a, b, op), reads, writes)

        def ts(eng, o, a, s1, s2, op0, op1, reads, writes):
            if s2 is None:
                fw.op(eng, lambda e: e.tensor_scalar(o, a, s1, None, op0), reads, writes)
            else:
                fw.op(eng, lambda e: e.tensor_scalar(o, a, s1, s2, op0, op1), reads, writes)

        def end_phase():
            fw.barrier()
            with nc.Block() as blk:
                fw.emit(blk)
            for k in fw.prog:
                fw.prog[k] = []

        def stt(eng, o, a, s, b, op0, op1, reads, writes):
            fw.op(eng, lambda e: e.scalar_tensor_tensor(o, a, s, b, op0, op1), reads, writes)

        def cp(eng, o, i, reads, writes):
            if eng == "act":
                fw.op("act", lambda e: e.copy(o, i), reads, writes)
            else:
                fw.op(eng, lambda e: e.tensor_copy(o, i), reads, writes)

        def recip(o, i, reads, writes):
            fw.op("dve", lambda e: e.reciprocal(o, i), reads, writes)

        def ld(o, i, writes, reads=()):
            fw.dma("sp", o, i, reads=reads, writes=writes)

        def st_(o, i, reads, writes=()):
            fw.dma("sp", o, i, reads=reads, writes=writes)

        IDENT = sbt(gst, "IDENT", [128, 128], F32); dIDENT = Dep()
        IOTA = sbt(gst, "IOTA", [128, 128], F32); dIOTA = Dep()
        ONES32 = sbt(gst, "ONES32", [128, 128], F32); dONES32 = Dep()
        ONES16 = sbt(gst, "ONES16", [128, 128], BF16); dONES16 = Dep()
        MODT = sbt(gst, "MODT", [128, 48, 2], F32); dMODT = Dep()
        SC1P = sbt(gst, "SC1P", [128, 8, 2], F32); dSC1P = Dep()
        NLAM = sbt(gst, "NLAM", [128, 1], F32); dNLAM = Dep()
        COLS = sbt(gst, "COLS", [128, 5], F32); dCOLS = Dep()
        SGC = sbt(gst, "SGC", [128, 1], F32); dSGC = Dep()
        ld(IDENT[:], ident_d, [dIDENT]); ld(IOTA[:], iota_d, [dIOTA]); ld(COLS[:], colsm, [dCOLS])
        fw.op("dve", lambda e: e.memset(ONES32[:], 1.0), (), [dONES32])
        fw.op("dve", lambda e: e.memset(ONES16[:], 1.0), (), [dONES16])
        fw.op("dve", lambda e: e.tensor_scalar(SGC[:], COLS[:, 4:5], 1.0 - LAMBDA_INIT, None, ALU.mult), [dCOLS], [dSGC])

        with ExitStack() as st:
            BCT = sbt(st, "BCT", [128, 8, 1024], F32); dBCT = Dep()
            CT = sbt(st, "CT", [128, 16], F32); dCT = Dep()
            SL = sbt(st, "SL", [128, 8, 2], F32); dSL = Dep()
            BCOL = sbt(st, "BCOL", [128, 48], F32); dBCOL = Dep()
            MODR = sbt(st, "MODR", [2, 6144], F32); dMODR = Dep()
            BROW = sbt(st, "BROW", [2, 6144], F32); dBROW = Dep()
            ROWS = sbt(st, "ROWS", [1, 4096], F32); dROWS = Dep()
            LAMR = sbt(st, "LAMR", [1, 256], F32); dLAMR = Dep()
            LAMB = sbt(st, "LAMB", [128, 256], F32); dLAMB = Dep()
            LTMP = sbt(st, "LTMP", [128, 4], F32); dLTMP = Dep()
            WM = [sbt(st, "WM%d" % i, [128, 8, 512], F32) for i in range(2)]; dWM = [Dep(), Dep()]
            ld(CT[:], cT, [dCT]); ld(BCOL[:], b_col, [dBCOL])
            ld(BROW[0:1, :], b_row, [dBROW]); ld(BROW[1:2, :], b_row, [dBROW])
            ld(ROWS[:], rows4, [dROWS]); ld(LAMR[:], lamrow, [dLAMR])
            act(SL[:, :, 0], CT[:, 0:8], ACTF.Silu, [dCT], [dSL])
            act(SL[:, :, 1], CT[:, 8:16], ACTF.Silu, [dCT], [dSL])
            wmv = w_mod.rearrange("(k p) n -> p k n", p=128)
            for j in range(12):
                W_ = WM[j % 2]; dW_ = dWM[j % 2]
                ld(W_[:], wmv[:, :, j * 512:(j + 1) * 512], [dW_])
                for cc in range(4):
                    for k in range(8):
                        mm(PB_[0][:, cc * 2:cc * 2 + 2], W_[:, k, cc * 128:(cc + 1) * 128], SL[:, k, :],
                           k == 0, k == 7, [dW_, dSL], [dB[0]])
                tt("dve", MODT[:, j * 4:(j + 1) * 4, :], PB_[0][:, 0:8].rearrange("p (c j) -> p c j", j=2),
                   BCOL[:, j * 4:(j + 1) * 4].unsqueeze(2).to_broadcast([128, 4, 2]), ALU.add, [dB[0], dBCOL], [dMODT])
                for k in range(8):
                    mm(PB_[1][0:2, :], SL[:, k, :], W_[:, k, :], k == 0, k == 7, [dW_, dSL], [dB[1]])
                tt("dve", MODR[:, j * 512:(j + 1) * 512], PB_[1][0:2, :], BROW[:, j * 512:(j + 1) * 512], ALU.add,
                   [dB[1], dBROW], [dMODR])
            ts("dve", SC1P[:], MODT[:, 8:16, :], 1.0, None, ALU.add, ALU.bypass, [dMODT], [dSC1P])
            srcs = [(MODR, 2048, dMODR), (MODR, 3072, dMODR), (MODR, 4096, dMODR), (MODR, 5120, dMODR),
                    (ROWS, 0, dROWS), (ROWS, 1024, dROWS), (ROWS, 2048, dROWS), (ROWS, 3072, dROWS)]
            for i, (src, off, dsrc) in enumerate(srcs):
                for hf in range(2):
                    bk = 2 + hf
                    mm(PB_[bk][:], ONES32[0:1, :], src[0:1, off + hf * 512: off + (hf + 1) * 512], True, True,
                       [dONES32, dsrc], [dB[bk]])
                    if i == 2:
                        ts("dve", BCT[:, i, hf * 512:(hf + 1) * 512], PB_[bk][:], 1.0, None, ALU.add, ALU.bypass,
                           [dB[bk]], [dBCT])
                    else:
                        cp("dve", BCT[:, i, hf * 512:(hf + 1) * 512], PB_[bk][:], [dB[bk]], [dBCT])
            st_(bctd, BCT[:], [dBCT])
            mm(PB_[4][:, 0:256], ONES32[0:1, :], LAMR[0:1, :], True, True, [dONES32, dLAMR], [dB[4]])
            cp("dve", LAMB[:], PB_[4][:, 0:256], [dB[4]], [dLAMB])
            tt("dve", LAMB[:, 0:64], LAMB[:, 0:64], LAMB[:, 64:128], ALU.mult, [dLAMB], [dLAMB])
            tt("dve", LAMB[:, 128:192], LAMB[:, 128:192], LAMB[:, 192:256], ALU.mult, [dLAMB], [dLAMB])
            fw.op("dve", lambda e: e.reduce_sum(LTMP[:, 0:1], LAMB[:, 0:64], AX.X), [dLAMB], [dLTMP])
            fw.op("dve", lambda e: e.reduce_sum(LTMP[:, 1:2], LAMB[:, 128:192], AX.X), [dLAMB], [dLTMP])
            act(LTMP[:, 2:4], LTMP[:, 0:2], ACTF.Exp, [dLTMP], [dLTMP])
            tt("dve", NLAM[:], LTMP[:, 3:4], LTMP[:, 2:3], ALU.subtract, [dLTMP], [dNLAM])
            ts("dve", NLAM[:], NLAM[:], -LAMBDA_INIT, None, ALU.add, ALU.bypass, [dNLAM], [dNLAM])
            end_phase()

        with ExitStack() as ast:
            HK = sbt(ast, "HK", [128, 8, NK], BF16); dHK = Dep()
            with ExitStack() as st:
                XT = [sbt(st, "XT%d" % i, [128, D], F32) for i in range(2)]; dXT = [Dep(), Dep()]
                for i in range(NKT):
                    X_ = XT[i % 2]; dX_ = dXT[i % 2]
                    src = ctx[i * 128:(i + 1) * 128, :] if i < 2 else x[(i - 2) * 128:(i - 1) * 128, :]
                    j = 1 if i < 2 else 0
                    ld(X_[:], src, [dX_])
                    for k in range(8):
                        bk = k // 4
                        tr(PB_[bk][:, (k % 4) * 128:(k % 4 + 1) * 128], X_[:, k * 128:(k + 1) * 128], IDENT[:],
                           [dX_, dIDENT], [dB[bk]])
                    for k in range(8):
                        bk = k // 4
                        ts("dve", HK[:, k, i * 128:(i + 1) * 128], PB_[bk][:, (k % 4) * 128:(k % 4 + 1) * 128],
                           SC1P[:, k, j:j + 1], MODT[:, k, j:j + 1], ALU.mult, ALU.add, [dB[bk], dSC1P, dMODT], [dHK])
                end_phase()

            with ExitStack() as rst:
                TCc = [sbt(rst, "TCc%d" % i, [128, 512], F32) for i in range(2)]; dTCc = [Dep(), Dep()]
                TSc = [sbt(rst, "TSc%d" % i, [128, 512], F32) for i in range(2)]; dTSc = [Dep(), Dep()]
                tabn = [0]

                def load_tab(np_, to, sl):
                    i = tabn[0] % 2; tabn[0] += 1
                    ld(TCc[i][0:np_, 0:sl], tabC[0:np_, to:to + sl], [dTCc[i]])
                    ld(TSc[i][0:np_, 0:sl], tabS[0:np_, to:to + sl], [dTSc[i]])
                    return TCc[i], dTCc[i], TSc[i], dTSc[i]
                QNT = sbt(rst, "QNT", [128, 2, T], BF16); dQNT = Dep()
                KVNT = sbt(rst, "KVNT", [128, 2, NK], BF16); dKVNT = Dep()
                KR = sbt(rst, "KR", [128, NK], BF16); dKR = Dep()
                fw.op("pool", lambda e: e.memset(KR[64:128, :], 0.0), (), [dKR])
                chunks_k = [(c * 512, 512) for c in range(8)] + [(4096, 256)]
                with ExitStack() as st:
                    WL32 = sbt(st, "WL32", [128, 8, 640], F32); dWL32 = Dep()
                    WL = sbt(st, "WL", [128, 8, 640], BF16); dWL = Dep()
                    SQ = sbt(st, "SQ", [128, 2, 512], F32); dSQ = Dep()
                    RS = sbt(st, "RS", [128, 512], F32); dRS = Dep()
                    T1 = sbt(st, "T1", [64, 512], F32); dT1 = Dep()
                    T2 = sbt(st, "T2", [64, 512], F32); dT2 = Dep()
                    ld(WL32[:], w_lat.rearrange("(k p) n -> p k n", p=128), [dWL32])
                    cp("act", WL[:], WL32[:], [dWL32], [dWL])

                    def latent(colbase, gcol, dst, ddst, koff, klen, dcol):
                        for m in range(2):
                            for k in range(8):
                                mm(PB_[m][:, 0:klen], WL[:, k, colbase + m * 128: colbase + (m + 1) * 128],
                                   HK[:, k, koff:koff + klen], k == 0, k == 7, [dWL, dHK], [dB[m]])
                            act(SQ[:, m, 0:klen], PB_[m][:, 0:klen], ACTF.Square, [dB[m]], [dSQ])
                        for m in range(2):
                            mm(PB_[2][:, 0:klen], ONES32[:], SQ[:, m, 0:klen], m == 0, m == 1, [dONES32, dSQ], [dB[2]])
                        act(RS[:, 0:klen], PB_[2][:, 0:klen], ACTF.Sqrt, [dB[2]], [dRS], bias=EPS, scale=1.0 / 256.0)
                        recip(RS[:, 0:klen], RS[:, 0:klen], [dRS], [dRS])
                        for m in range(2):
                            stt("dve", dst[:, m, dcol:dcol + klen], PB_[m][:, 0:klen], COLS[:, gcol + m:gcol + m + 1],
                                RS[:, 0:klen], ALU.mult, ALU.mult, [dB[m], dCOLS, dRS], [ddst])

                    for (ko, kl) in chunks_k:
                        latent(256, 2, KVNT, dKVNT, ko, kl, ko)
                        for k in range(8):
                            mm(PB_[3][0:64, 0:kl], WL[:, k, 512:576], HK[:, k, ko:ko + kl], k == 0, k == 7, [dWL, dHK], [dB[3]])
                        for k in range(8):
                            mm(PB_[4][0:64, 0:kl], WL[:, k, 576:640], HK[:, k, ko:ko + kl], k == 0, k == 7, [dWL, dHK], [dB[4]])
                        TC_, dTC_, TS_, dTS_ = load_tab(64, ko, kl)
                        tt("dve", T1[:, 0:kl], PB_[3][0:64, 0:kl], TC_[0:64, 0:kl], ALU.mult, [dB[3], dTC_], [dT1])
                        tt("dve", T2[:, 0:kl], PB_[4][0:64, 0:kl], TS_[0:64, 0:kl], ALU.mult, [dB[4], dTS_], [dT2])
                        tt("pool", KR[0:64, ko:ko + kl], T1[:, 0:kl], T2[:, 0:kl], ALU.add, [dT1, dT2], [dKR])
                    for c in range(8):
                        latent(0, 0, QNT, dQNT, NCTX + c * 512, 512, c * 512)
                    end_phase()
                with ExitStack() as st:
                    PT = [sbt(st, "PT%d" % i, [128, 512], BF16) for i in range(3)]; dPT = [Dep() for _ in range(3)]
                    RZ = sbt(st, "RZ", [128, 512], F32); dRZ = Dep()
                    ZA = sbt(st, "ZA", [128, 512], F32); dZA = Dep()
                    T1 = [sbt(st, "T1_%d" % i, [128, 512], F32) for i in range(1)]; dT1 = [Dep(), Dep()]
                    T2 = [sbt(st, "T2_%d" % i, [128, 512], F32) for i in range(1)]; dT2 = [Dep(), Dep()]
                    QH = [sbt(st, "QH%d" % i, [128, T], BF16) for i in range(1)]; dQH = [Dep(), Dep()]
                    KH = [sbt(st, "KH%d" % i, [128, NK], BF16) for i in range(1)]; dKH = [Dep(), Dep()]
                    VH = [sbt(st, "VH%d" % i, [128, NKT, 128], BF16) for i in range(1)]; dVH = [Dep(), Dep()]
                    AUX = sbt(st, "AUX", [128, NK], BF16); dAUX = Dep()
                    QRH = [AUX]; dQRH = [dAUX, dAUX]
                    WS = [sbt(st, "WS%d" % i, [128, 8, 128], F32) for i in range(1)]; dWS = [Dep(), Dep()]
                    WH = [sbt(st, "WH%d" % i, [128, 8, 640], BF16) for i in range(1)]; dWH = [Dep(), Dep()]
                    OM = [sbt(st, "OM%d" % i, [128, 512], F32) for i in range(2)]; dOM = [Dep(), Dep()]
                    DH = sbt(st, "DH", [128, 512], F32); dDH = Dep()
                    SQ2 = sbt(st, "SQ2", [128, 512], F32); dSQ2 = Dep()
                    RS2 = sbt(st, "RS2", [128, 512], F32); dRS2 = Dep()
                    YO = [sbt(st, "YO%d" % i, [128, 512], BF16) for i in range(2)]; dYO = [Dep(), Dep()]
                    tcount = [0]
                    SBK = [0, 1, 2, 7]

                    blocks = []

                    def attn(Sfn, Vt, dV, scale, si, consume, zdve=False, post=None):
                        blocks.append((Sfn, Vt, dV, scale, si, consume, zdve, post))

                    def run_blocks():
                        steps = [(bi, kt) for bi in range(len(blocks)) for kt in range(NKT)]

                        def emitS(j):
                            if j < len(steps):
                                bi_, kt_ = steps[j]
                                blocks[bi_][0](kt_, j)
                        emitS(0)
                        emitS(1)
                        for j, (bi, kt) in enumerate(steps):
                            emitS(j + 2)
                            Sfn, Vt, dV, scale, si, consume, zdve, post = blocks[bi]
                            PO = PB_[3 + 2 * si]; dPO = dB[3 + 2 * si]; PZ = PB_[4 + 2 * si]; dPZ = dB[4 + 2 * si]
                            b = j % 3
                            sb_ = SBK[j % 4]
                            act(PT[b][:], PB_[sb_][:], ACTF.Exp, [dB[sb_]], [dPT[b]], scale=scale)
                            mm(PO[:], Vt[:, kt, :], PT[b][:], kt == 0, kt == NKT - 1, [dV, dPT[b]], [dPO])
                            if not zdve:
                                mm(PZ[:], ONES16[:], PT[b][:], kt == 0, kt == NKT - 1, [dONES16, dPT[b]], [dPZ])
                            elif kt == 0:
                                cp("dve", ZA[:], PT[b][:], [dPT[b]], [dZA])
                            else:
                                tt("dve", ZA[:], ZA[:], PT[b][:], ALU.add, [dZA, dPT[b]], [dZA])
                            if kt == NKT - 1:
                                if zdve:
                                    mm(PZ[:], ONES32[:], ZA[:], True, True, [dONES32, dZA], [dPZ])
                                recip(RZ[:], PZ[:], [dPZ], [dRZ])
                                consume(PO, dPO)
                                if post is not None:
                                    post()
                        del blocks[:]

                    def rope_proj(W_, dW_, ca, cb, src, dsrc, so, sl, to, dst, ddst, do, np_, dst2=None, ddst2=None):
                        nk_ = src.shape[1]
                        for k in range(nk_):
                            mm(PB_[0][0:np_, 0:sl], W_[:, k, ca:ca + np_], src[:, k, so:so + sl], k == 0, k == nk_ - 1,
                               [dW_, dsrc], [dB[0]])
                        for k in range(nk_):
                            mm(PB_[1][0:np_, 0:sl], W_[:, k, cb:cb + np_], src[:, k, so:so + sl], k == 0, k == nk_ - 1,
                               [dW_, dsrc], [dB[1]])
                        i = 0
                        TC_, dTC_, TS_, dTS_ = load_tab(np_, to, sl)
                        tt("dve", T1[i][0:np_, 0:sl], PB_[0][0:np_, 0:sl], TC_[0:np_, 0:sl], ALU.mult, [dB[0], dTC_], [dT1[i]])
                        tt("dve", T2[i][0:np_, 0:sl], PB_[1][0:np_, 0:sl], TS_[0:np_, 0:sl], ALU.mult, [dB[1], dTS_], [dT2[i]])
                        if dst2 is None:
                            tt("pool", dst[0:np_, do:do + sl], T1[i][0:np_, 0:sl], T2[i][0:np_, 0:sl], ALU.add, [dT1[i], dT2[i]], [ddst])
                        else:
                            tt("pool", dst[0:64, do:do + sl], T1[i][0:64, 0:sl], T2[i][0:64, 0:sl], ALU.add, [dT1[i], dT2[i]], [ddst])
                            tt("pool", dst2[64:128, do:do + sl], T1[i][64:128, 0:sl], T2[i][64:128, 0:sl], ALU.add, [dT1[i], dT2[i]], [ddst2])

                    def vproj(W_, dW_, c0, src, dsrc, Vt, dVt):
                        nk_ = src.shape[1]
                        for g0 in range(0, NKT, 4):
                            n = min(4, NKT - g0)
                            for j in range(n):
                                kt = g0 + j
                                for k in range(nk_):
                                    mm(PB_[7][:, j * 128:(j + 1) * 128], src[:, k, kt * 128:(kt + 1) * 128], W_[:, k, c0:c0 + 128],
                                       k == 0, k == nk_ - 1, [dsrc, dW_], [dB[7]])
                            cp("act", Vt[:, g0:g0 + n, :], PB_[7][:, 0:n * 128].rearrange("p (a b) -> p a b", b=128), [dB[7]], [dVt])

                    cast_jobs = []
                    for i in range(32):
                        cast_jobs.append((uTb_r[i * 2:(i + 1) * 2], uT[i * 2:(i + 1) * 2]))
                    for i in range(32):
                        cast_jobs.append((vRb[i * 512:(i + 1) * 512, :], vR[i * 512:(i + 1) * 512, :]))
                    for i in range(2):
                        cast_jobs.append((wpqb[i * 512:(i + 1) * 512, :], w_pq[i * 512:(i + 1) * 512, :]))

                    def cast_step(n):
                        for _ in range(n):
                            if cast_jobs:
                                o_, i_ = cast_jobs.pop(0)
                                fw.dma("pool", o_, i_)
                    fw.op("pool", lambda e: e.memset(KH[0][64:128, :], 0.0), (), [dKH[0]])
                    fw.op("pool", lambda e: e.memset(AUX[0:64, :], 0.0), (), [dAUX])
                    if "diff" not in skip:
                      for h in range(8):
                        hb = 0
                        cast_step(5)
                        wv = w_da[h].rearrange("(k p) n -> p k n", p=128)
                        for pc in range(5):
                            ld(WS[0][:], wv[:, :, pc * 128:(pc + 1) * 128], [dWS[0]])
                            cp("dve", WH[hb][:, :, pc * 128:(pc + 1) * 128], WS[0][:], [dWS[0]], [dWH[hb]])
                        for qc in range(8):
                            rope_proj(WH[hb], dWH[hb], 0, 128, HK, dHK, NCTX + qc * 512, 512, NCTX + qc * 512, QH[hb], dQH[hb], qc * 512, 128)
                        for (ko, kl) in chunks_k:
                            rope_proj(WH[hb], dWH[hb], 256, 384, HK, dHK, ko, kl, ko, KH[hb], dKH[hb], ko, 128, AUX, dAUX)
                        vproj(WH[hb], dWH[hb], 512, HK, dHK, VH[hb], dVH[hb])
                        for qc in range(8):
                            for m in range(2):
                                def Sfn(kt, j, m=m, qc=qc):
                                    Km = KH[hb] if m == 0 else AUX
                                    mm(PB_[SBK[j % 4]][:], Km[:, kt * 128:(kt + 1) * 128],
                                       QH[hb][:, qc * 512:(qc + 1) * 512], True, True, [dKH[hb], dAUX, dQH[hb]], [dB[SBK[j % 4]]])

                                def consume(PO, dPO, m=m):
                                    tt("dve", OM[m][:], PO[:], RZ[:], ALU.mult, [dPO, dRZ], [dOM[m]])

                                def post(qc=qc, h=h):
                                    stt("dve", DH[:], OM[1][:], NLAM[:, 0:1], OM[0][:], ALU.mult, ALU.add,
                                        [dOM[0], dOM[1], dNLAM], [dDH])
                                    sl_ = slice(qc * 512, (qc + 1) * 512)
                                    act(SQ2[:], DH[:], ACTF.Square, [dDH], [dSQ2])
                                    mm(PB_[6][:], ONES32[:], SQ2[:], True, True, [dONES32, dSQ2], [dB[6]])
                                    act(RS2[:], PB_[6][:], ACTF.Sqrt, [dB[6]], [dRS2], bias=EPS, scale=1.0 / 128.0)
                                    recip(RS2[:], RS2[:], [dRS2], [dRS2])
                                    stt("dve", YO[qc % 2][:], DH[:], SGC[:, 0:1], RS2[:], ALU.mult, ALU.mult, [dDH, dSGC, dRS2], [dYO[qc % 2]])
                                    st_(yad[h, :, sl_], YO[qc % 2][:], [dYO[qc % 2]])
                                attn(Sfn, VH[hb], dVH[hb], 0.125, m, consume, post=(post if m == 1 else None))
                        run_blocks()

                    WM32 = [sbt(st, "WM32_%d" % i, [128, 2, 512], F32) for i in range(1)]; dWM32 = [Dep(), Dep()]
                    WMb = [sbt(st, "WMb%d" % i, [128, 2, 512], BF16) for i in range(1)]; dWMb = [Dep(), Dep()]
                    if "mla" not in skip:
                      for h in range(8):
                        hb = 0
                        cast_step(5)
                        ld(WM32[hb][:], w_mla[h].rearrange("(k p) n -> p k n", p=128), [dWM32[hb]])
                        cp("dve", WMb[hb][:], WM32[hb][:], [dWM32[hb]], [dWMb[hb]])
                        for qc in range(8):
                            for kk in range(2):
                                mm(PB_[2][:], WMb[hb][:, kk, 0:128], QNT[:, kk, qc * 512:(qc + 1) * 512], kk == 0, kk == 1,
                                   [dWMb[hb], dQNT], [dB[2]])
                            cp("act", QH[hb][:, qc * 512:(qc + 1) * 512], PB_[2][:], [dB[2]], [dQH[hb]])
                            rope_proj(WMb[hb], dWMb[hb], 128, 192, QNT, dQNT, qc * 512, 512, NCTX + qc * 512, QRH[hb], dQRH[hb], qc * 512, 64)
                        for (ko, kl) in chunks_k:
                            for kk in range(2):
                                mm(PB_[2][:, 0:kl], WMb[hb][:, kk, 256:384], KVNT[:, kk, ko:ko + kl], kk == 0, kk == 1,
                                   [dWMb[hb], dKVNT], [dB[2]])
                            cp("act", KH[hb][:, ko:ko + kl], PB_[2][:, 0:kl], [dB[2]], [dKH[hb]])
                        vproj(WMb[hb], dWMb[hb], 384, KVNT, dKVNT, VH[hb], dVH[hb])
                        for qc in range(8):
                            def Sfn(kt, j, qc=qc):
                                mm(PB_[SBK[j % 4]][:], KH[hb][:, kt * 128:(kt + 1) * 128], QH[hb][:, qc * 512:(qc + 1) * 512],
                                   True, False, [dKH[hb], dQH[hb]], [dB[SBK[j % 4]]])
                                mm(PB_[SBK[j % 4]][:], KR[:, kt * 128:(kt + 1) * 128], QRH[hb][:, qc * 512:(qc + 1) * 512],
                                   False, True, [dKR, dQRH[hb]], [dB[SBK[j % 4]]])

                            def consume(PO, dPO, qc=qc, h=h):
                                tt("dve", YO[qc % 2][:], PO[:], RZ[:], ALU.mult, [dPO, dRZ], [dYO[qc % 2]])
                                st_(ybd[h, :, qc * 512:(qc + 1) * 512], YO[qc % 2][:], [dYO[qc % 2]])
                            attn(Sfn, VH[hb], dVH[hb], 192.0 ** -0.5, qc % 2, consume, zdve=True)
                        run_blocks()
                    cast_step(100)
                    end_phase()
            with ExitStack() as st:
                WST = [sbt(st, "WST%d" % i, [128, 8, 256], F32) for i in range(2)]; dWST = [Dep(), Dep()]
                WPA = sbt(st, "WPA", [128, 8, D], BF16); dWPA = Dep()
                WPB = sbt(st, "WPB", [128, 8, D], BF16); dWPB = Dep()
                WG = sbt(st, "WG", [128, 8, 2048], BF16); dWG = Dep()
                YA = [sbt(st, "YA%d" % i, [128, 8, 512], BF16) for i in range(2)]; dYA = [Dep(), Dep()]
                YB = [sbt(st, "YB%d" % i, [128, 8, 512], BF16) for i in range(2)]; dYB = [Dep(), Dep()]
                GA = sbt(st, "GA", [128, 512], F32); dGA = Dep()
                GB = sbt(st, "GB", [128, 512], F32); dGB = Dep()
                U1 = sbt(st, "U1", [128, 512], F32); dU1 = Dep()
                U2 = sbt(st, "U2", [128, 512], F32); dU2 = Dep()
                ZT = [sbt(st, "ZT%d" % i, [128, 8, 512], BF16) for i in range(2)]; dZT = [Dep(), Dep()]
                wcnt = [0]

                def load_w(dst, ddst, src, ncols):
                    sv = src.rearrange("(k p) n -> p k n", p=128)
                    for c0 in range(0, ncols, 256):
                        i = wcnt[0] % 2; wcnt[0] += 1
                        ld(WST[i][:], sv[:, :, c0:c0 + 256], [dWST[i]])
                        cp("dve" if (wcnt[0] % 2) else "act", dst[:, :, c0:c0 + 256], WST[i][:], [dWST[i]], [ddst])
                load_w(WPA, dWPA, w_pa, D); load_w(WPB, dWPB, w_pb, D); load_w(WG, dWG, w_gate, 2048)
                for qc in range(8):
                    qb = qc % 2
                    sl_ = slice(qc * 512, (qc + 1) * 512)
                    ld(YA[qb][:], yad[:, :, sl_].rearrange("h p t -> p h t"), [dYA[qb]])
                    ld(YB[qb][:], ybd[:, :, sl_].rearrange("h p t -> p h t"), [dYB[qb]])
                    for oc in range(8):
                        oc_ = slice(oc * 128, (oc + 1) * 128)
                        for hh in range(8):
                            mm(PB_[0][:], WPA[:, hh, oc_], YA[qb][:, hh, :], hh == 0, hh == 7, [dWPA, dYA[qb]], [dB[0]])
                        for hh in range(8):
                            mm(PB_[1][:], WPB[:, hh, oc_], YB[qb][:, hh, :], hh == 0, hh == 7, [dWPB, dYB[qb]], [dB[1]])
                        for k in range(8):
                            mm(PB_[2][:], WG[:, k, oc * 128:(oc + 1) * 128], HK[:, k, NCTX + qc * 512:NCTX + (qc + 1) * 512],
                               k == 0, k == 7, [dWG, dHK], [dB[2]])
                        for k in range(8):
                            mm(PB_[3][:], WG[:, k, 1024 + oc * 128:1024 + (oc + 1) * 128], HK[:, k, NCTX + qc * 512:NCTX + (qc + 1) * 512],
                               k == 0, k == 7, [dWG, dHK], [dB[3]])
                        act(GA[:], PB_[2][:], ACTF.Sigmoid, [dB[2]], [dGA])
                        act(GB[:], PB_[3][:], ACTF.Sigmoid, [dB[3]], [dGB])
                        tt("dve", U1[:], PB_[0][:], GA[:], ALU.mult, [dB[0], dGA], [dU1])
                        tt("dve", U2[:], PB_[1][:], GB[:], ALU.mult, [dB[1], dGB], [dU2])
                        tt("pool", ZT[qb][:, oc, :], U1[:], U2[:], ALU.add, [dU1, dU2], [dZT[qb]])
                    st_(zTd[:, :, sl_], ZT[qb][:], [dZT[qb]])
                end_phase()
        with ExitStack() as st:
            WST = [sbt(st, "WSTb%d" % i, [128, 8, 256], F32) for i in range(2)]; dWST = [Dep(), Dep()]
            WOUT = sbt(st, "WOUT", [128, 8, D], BF16); dWOUT = Dep()
            BC = sbt(st, "BC5", [128, 5, D], F32); dBC = Dep()
            ZC = [sbt(st, "ZC%d" % i, [128, 8, 512], BF16) for i in range(2)]; dZC = [Dep(), Dep()]
            XR = [sbt(st, "XR%d" % i, [128, D], F32) for i in range(2)]; dXR = [Dep(), Dep()]
            TMP = sbt(st, "TMP", [128, D], F32); dTMP = Dep()
            PRE = sbt(st, "PRE", [128, D], F32); dPRE = Dep()
            JNK = sbt(st, "JNK", [128, D], F32); dJNK = Dep()
            X1 = [sbt(st, "X1_%d" % i, [128, D], F32) for i in range(2)]; dX1 = [Dep(), Dep()]
            H2 = sbt(st, "H2", [128, D], F32); dH2 = Dep()
            H2T = [sbt(st, "H2T%d" % i, [128, 8, 128], BF16) for i in range(2)]; dH2T = [Dep(), Dep()]
            STA = sbt(st, "STA", [128, 8], F32); dSTA = Dep()
            sv = w_out.rearrange("(k p) n -> p k n", p=128)
            for c0 in range(0, D, 256):
                i = (c0 // 256) % 2
                ld(WST[i][:], sv[:, :, c0:c0 + 256], [dWST[i]])
                cp("dve", WOUT[:, :, c0:c0 + 256], WST[i][:], [dWST[i]], [dWOUT])
            for i, j in enumerate([0, 1, 2, 4, 5]):
                ld(BC[:, i, :], bctd[:, j, :], [dBC])

            def layer_norm(PREt, dPREt, gi, bi, OUTt, dOUTt, BCt, dBCt):
                act(JNK[:], PREt[:], ACTF.Identity, [dPREt], [dJNK, dSTA], accum=STA[:, 0:1])
                act(JNK[:], PREt[:], ACTF.Square, [dPREt], [dJNK, dSTA], accum=STA[:, 1:2])
                ts("dve", STA[:, 2:3], STA[:, 0:1], 1.0 / D, None, ALU.mult, None, [dSTA], [dSTA])
                tt("dve", STA[:, 3:4], STA[:, 2:3], STA[:, 2:3], ALU.mult, [dSTA], [dSTA])
                stt("dve", STA[:, 4:5], STA[:, 1:2], 1.0 / D, STA[:, 3:4], ALU.mult, ALU.subtract, [dSTA], [dSTA])
                act(STA[:, 5:6], STA[:, 4:5], ACTF.Sqrt, [dSTA], [dSTA], bias=EPS, scale=1.0)
                recip(STA[:, 6:7], STA[:, 5:6], [dSTA], [dSTA])
                ts("dve", OUTt[:], PREt[:], STA[:, 2:3], STA[:, 6:7], ALU.subtract, ALU.mult, [dPREt, dSTA], [dOUTt])
                tt("pool", OUTt[:], OUTt[:], BCt[:, gi, :], ALU.mult, [dOUTt, dBCt], [dOUTt])
                tt("pool", OUTt[:], OUTt[:], BCt[:, bi, :], ALU.add, [dOUTt, dBCt], [dOUTt])

            for qc in range(8):
                qb = qc % 2
                ld(ZC[qb][:], zTd[:, :, qc * 512:(qc + 1) * 512], [dZC[qb]])
                for t4 in range(4):
                    ti = qc * 4 + t4
                    xb = ti % 2
                    ld(XR[xb][:], x[ti * 128:(ti + 1) * 128, :], [dXR[xb]])
                    for hf in range(2):
                        for oc in range(8):
                            mm(PB_[hf][:], ZC[qb][:, oc, t4 * 128:(t4 + 1) * 128], WOUT[:, oc, hf * 512:(hf + 1) * 512],
                               oc == 0, oc == 7, [dZC[qb], dWOUT], [dB[hf]])
                        tt("dve", TMP[:, hf * 512:(hf + 1) * 512], PB_[hf][:], BC[:, 0, hf * 512:(hf + 1) * 512], ALU.mult,
                           [dB[hf], dBC], [dTMP])
                    stt("dve", PRE[:], XR[xb][:], ALPHA, TMP[:], ALU.mult, ALU.add, [dXR[xb], dTMP], [dPRE])
                    layer_norm(PRE, dPRE, 3, 4, X1[xb], dX1[xb], BC, dBC)
                    st_(x1d[ti * 128:(ti + 1) * 128, :], X1[xb][:], [dX1[xb]])
                    tt("dve", H2[:], X1[xb][:], BC[:, 2, :], ALU.mult, [dX1[xb], dBC], [dH2])
                    tt("pool", H2[:], H2[:], BC[:, 1, :], ALU.add, [dH2, dBC], [dH2])
                    for k in range(8):
                        bk = 2 + k // 4
                        tr(PB_[bk][:, (k % 4) * 128:(k % 4 + 1) * 128], H2[:, k * 128:(k + 1) * 128], IDENT[:], [dH2, dIDENT], [dB[bk]])
                    for g in range(2):
                        cp("act", H2T[xb][:, g * 4:(g + 1) * 4, :], PB_[2 + g][:].rearrange("p (a b) -> p a b", b=128), [dB[2 + g]], [dH2T[xb]])
                    st_(h2Td[:, :, ti * 128:(ti + 1) * 128], H2T[xb][:], [dH2T[xb]])
            end_phase()
        with ExitStack() as st:
            GG = sbt(st, "GG", [128, 128, 256], BF16); dGG = Dep()
            KEYSb = sbt(st, "KEYSb", [128, 16, 128], BF16); dKEYS = Dep()
            BC7 = sbt(st, "BC7", [128, 3, D], F32); dBC7 = Dep()
            H2C = [sbt(st, "H2C%d" % i, [128, 8, 256], BF16) for i in range(2)]; dH2C = [Dep(), Dep()]
            WQ = [sbt(st, "WQ%d" % i, [128, 8, 128], BF16) for i in range(2)]; dWQ = [Dep(), Dep()]
            QPT = sbt(st, "QPT", [128, 16, 256], BF16); dQPT = Dep()
            SCB = [sbt(st, "SCB%d" % i, [128, 2048], F32) for i in range(2)]; dSCB = [Dep(), Dep()]
            BUFA = sbt(st, "BUFA", [128, 2048], F32); dBUFA = Dep()
            BUFB = sbt(st, "BUFB", [128, 2048], F32); dBUFB = Dep()
            V16 = sbt(st, "V16", [128, 16, 16], F32); dV16 = Dep()
            IX = sbt(st, "IX", [128, 16, 16], U32); dIX = Dep()
            IXF = sbt(st, "IXF", [128, 16, 16], F32); dIXF = Dep()
            CV = sbt(st, "CV", [128, 8, 16], F32); dCV = Dep()
            CPi = sbt(st, "CPi", [128, 8, 16], U32); dCPi = Dep()
            PF = sbt(st, "PF", [128, 8, 16], F32); dPF = Dep()
            AF = sbt(st, "AF", [128, 8, 16], F32); dAF = Dep()
            BF = sbt(st, "BF", [128, 8, 16], F32); dBF = Dep()
            SLT = [sbt(st, "SLT%d" % i, [128, 3, 128], F32) for i in range(2)]; dSLT = [Dep(), Dep()]
            EG = sbt(st, "EG", [128, 8, 16], F32); dEG = Dep()
            SG = sbt(st, "SG", [128, 8], F32); dSG = Dep()
            TR3 = [sbt(st, "TR3_%d" % i, [128, 384], F32) for i in range(2)]; dTR3 = [Dep(), Dep()]
            OH1 = [sbt(st, "OH1_%d" % i, [128, 16, 128], BF16) for i in range(2)]; dOH1 = [Dep(), Dep()]
            OH2 = [sbt(st, "OH2_%d" % i, [128, 16, 128], BF16) for i in range(2)]; dOH2 = [Dep(), Dep()]
            NUB = 3
            UG = [sbt(st, "UG%d" % i, [128, 8, 256], BF16) for i in range(NUB)]; dUG = [Dep() for _ in range(NUB)]
            VG = [sbt(st, "VG%d" % i, [128, 2, D], BF16) for i in range(NUB)]; dVG = [Dep() for _ in range(NUB)]
            GE = [sbt(st, "GE%d" % i, [128, 256], BF16) for i in range(2)]; dGE = [Dep(), Dep()]
            WT = [sbt(st, "WT%d" % i, [128, 256], BF16) for i in range(2)]; dWT = [Dep(), Dep()]
            TMP = sbt(st, "TMP7", [128, D], F32); dTMP = Dep()
            OUTt = sbt(st, "OUTt", [128, D], F32); dOUTt = Dep()
            X1t = OUTt; dX1t = dOUTt
            STA = sbt(st, "STA7", [128, 8], F32); dSTA = Dep()
            dB4h = [Dep(), Dep()]
            THR16 = sbt(st, "THR16", [128, 16], F32); dTHR16 = Dep()
            ts("dve", THR16[:], IOTA[:, 0:16], 16.0, None, ALU.mult, None, [dIOTA], [dTHR16])
            fw.dma("pool", KEYSb[:].rearrange("p a b -> p (a b)"), keysT, writes=[dKEYS])
            for i, j in enumerate([3, 6, 7]):
                ld(BC7[:, i, :], bctd[:, j, :], [dBC7])
            wqv = wpqb.rearrange("(k p) n -> p k n", p=128)
            WKv = BUFB[:].rearrange("p (a b) -> p a b", b=128)
            OHA = BUFB[:].rearrange("p (h a b) -> p h a b", a=16, b=16); dOHA = dBUFB
            CAND = BUFA[:].rearrange("p (h a b) -> p h a b", a=16, b=16)
            CANDf = BUFA[:].rearrange("p (h c) -> p h c", c=256)
            CWf = BUFB[:].rearrange("p (h c) -> p h c", c=256)
            V16v = V16[:].rearrange("p (h m) a -> p h m a", m=2)
            IXFv = IXF[:].rearrange("p (h m) a -> p h m a", m=2)
            B16 = [128, 8, 16, 16]
            ohc = [0]; gcn = [0]; ugn = [0]

            def ln7(PREt, dPREt, OUTt_, dOUTt_):
                act(OUTt_[:], PREt[:], ACTF.Identity, [dPREt], [dOUTt_, dSTA], accum=STA[:, 0:1])
                act(OUTt_[:], PREt[:], ACTF.Square, [dPREt], [dOUTt_, dSTA], accum=STA[:, 1:2])
                ts("dve", STA[:, 2:3], STA[:, 0:1], 1.0 / D, None, ALU.mult, None, [dSTA], [dSTA])
                tt("dve", STA[:, 3:4], STA[:, 2:3], STA[:, 2:3], ALU.mult, [dSTA], [dSTA])
                stt("dve", STA[:, 4:5], STA[:, 1:2], 1.0 / D, STA[:, 3:4], ALU.mult, ALU.subtract, [dSTA], [dSTA])
                act(STA[:, 5:6], STA[:, 4:5], ACTF.Sqrt, [dSTA], [dSTA], bias=EPS, scale=1.0)
                recip(STA[:, 6:7], STA[:, 5:6], [dSTA], [dSTA])
                ts("dve", OUTt_[:], PREt[:], STA[:, 2:3], STA[:, 6:7], ALU.subtract, ALU.mult, [dPREt, dSTA], [dOUTt_])
                tt("pool", OUTt_[:], OUTt_[:], BC7[:, 1, :], ALU.mult, [dOUTt_, dBC7], [dOUTt_])
                tt("pool", OUTt_[:], OUTt_[:], BC7[:, 2, :], ALU.add, [dOUTt_, dBC7], [dOUTt_])

            def top16_batch(n, vals, idxs, src, wk, dvals, didx, dsrc, dwk):
                for g in range(n):
                    fw.op("dve", lambda e, g=g: e.max(out=vals(g)[:, 0:8], in_=src(g)), [dsrc], [dvals])
                for g in range(n):
                    fw.op("dve", lambda e, g=g: e.max_index(out=idxs(g)[:, 0:8], in_max=vals(g)[:, 0:8], in_values=src(g)), [dsrc, dvals], [didx])
                for g in range(n):
                    fw.op("dve", lambda e, g=g: e.match_replace(out=wk(g), in_to_replace=vals(g)[:, 0:8], in_values=src(g), imm_value=NEG),
                          [dsrc, dvals], [dwk])
                for g in range(n):
                    fw.op("dve", lambda e, g=g: e.max(out=vals(g)[:, 8:16], in_=wk(g)), [dwk], [dvals])
                for g in range(n):
                    fw.op("dve", lambda e, g=g: e.max_index(out=idxs(g)[:, 8:16], in_max=vals(g)[:, 8:16], in_values=wk(g)), [dwk, dvals], [didx])

            def stageA_front(c):
                cb = c % 2
                ld(H2C[cb][:], h2Td[:, :, c * 256:(c + 1) * 256], [dH2C[cb]])
                for hm in range(16):
                    ld(WQ[hm % 2][:], wqv[:, :, hm * 128:(hm + 1) * 128], [dWQ[hm % 2]])
                    r = (hm % 2) * 256
                    for k in range(8):
                        mm(PB_[6][:, r:r + 256], WQ[hm % 2][:, k, :], H2C[cb][:, k, :], k == 0, k == 7, [dWQ[hm % 2], dH2C[cb]], [dB[6]])
                    if hm % 2 == 1:
                        cp("act", QPT[:, hm - 1:hm + 1, :], PB_[6][:].rearrange("p (a b) -> p a b", b=256), [dB[6]], [dQPT])
                for t2 in range(2):
                    SC = SCB[t2][:].rearrange("p (a b) -> p a b", b=128)
                    for g in range(4):
                        for j in range(4):
                            hm = g * 4 + j
                            mm(PB_[7][:, j * 128:(j + 1) * 128], QPT[:, hm, t2 * 128:(t2 + 1) * 128], KEYSb[:, hm, :], True, True,
                               [dQPT, dKEYS], [dB[7]])
                        cp("act", SCB[t2][:, g * 512:(g + 1) * 512], PB_[7][:], [dB[7]], [dSCB[t2]])
                for t2 in range(2):
                    SC = SCB[t2][:].rearrange("p (a b) -> p a b", b=128)
                    top16_batch(16, lambda g: V16[:, g, :], lambda g: IX[:, g, :], lambda g, SC=SC: SC[:, g, :], lambda g: WKv[:, g, :],
                                dV16, dIX, dSCB[t2], dBUFB)
                    cp("dve", IXF[:], IX[:], [dIX], [dIXF])
                    tt("dve", CAND, V16v[:, :, 0, :].unsqueeze(3).to_broadcast(B16), V16v[:, :, 1, :].unsqueeze(2).to_broadcast(B16),
                       ALU.add, [dV16], [dBUFA])
                    top16_batch(8, lambda g: CV[:, g, :], lambda g: CPi[:, g, :], lambda g: CANDf[:, g, :], lambda g: CWf[:, g, :],
                                dCV, dCPi, dBUFA, dBUFB)
                    cp("dve", PF[:], CPi[:], [dCPi], [dPF])
                    tt("dve", OHA, PF[:].unsqueeze(3).to_broadcast(B16), THR16[:, 0:16].unsqueeze(1).unsqueeze(1).to_broadcast(B16),
                       ALU.is_ge, [dPF, dTHR16], [dOHA])
                    fw.op("dve", lambda e: e.tensor_reduce(AF[:], OHA, AX.X, ALU.add), [dOHA], [dAF])
                    ts("dve", AF[:], AF[:], -1.0, None, ALU.add, None, [dAF], [dAF])
                    stt("dve", BF[:], AF[:], -16.0, PF[:], ALU.mult, ALU.add, [dAF, dPF], [dBF])
                    iob = IOTA[:, 0:16].unsqueeze(1).unsqueeze(1).to_broadcast(B16)
                    for (XF, dXF, mi) in ((AF, dAF, 0), (BF, dBF, 1)):
                        tt("dve", OHA, XF[:].unsqueeze(3).to_broadcast(B16), iob, ALU.is_equal, [dXF, dIOTA], [dOHA])
                        tt("dve", OHA, OHA, IXFv[:, :, mi, :].unsqueeze(2).to_broadcast(B16), ALU.mult, [dOHA, dIXF], [dOHA])
                        fw.op("dve", lambda e, mi=mi, t2=t2: e.tensor_reduce(SLT[t2][:, mi, :].rearrange("p (a b) -> p a b", b=16), OHA, AX.X, ALU.add),
                              [dOHA], [dSLT[t2]])
                    tt("dve", EG[:], CV[:], CV[:, :, 0:1].to_broadcast([128, 8, 16]), ALU.subtract, [dCV], [dEG])
                    act(EG[:], EG[:], ACTF.Exp, [dEG], [dEG])
                    fw.op("dve", lambda e: e.reduce_sum(SG[:], EG[:], AX.X), [dEG], [dSG])
                    recip(SG[:], SG[:], [dSG], [dSG])
                    tt("dve", SLT[t2][:, 2, :].rearrange("p (a b) -> p a b", b=16), EG[:], SG[:].unsqueeze(2).to_broadcast([128, 8, 16]), ALU.mult,
                       [dEG, dSG], [dSLT[t2]])

            def stageA_back(c):
                for t2 in range(2):
                    for i in range(3):
                        tr(PB_[6][:, i * 128:(i + 1) * 128], SLT[t2][:, i, :], IDENT[:], [dSLT[t2], dIDENT], [dB[6]])
                    cp("act", TR3[t2][:], PB_[6][:, 0:384], [dB[6]], [dTR3[t2]])

            def stageB(c):
                for t2 in range(2):
                    T3 = TR3[t2]; dT3 = dTR3[t2]
                    for s in range(8):
                        ob = ohc[0] % 2; ohc[0] += 1
                        t0 = s * 16
                        B3 = [128, 16, 128]
                        iot = IOTA[:].unsqueeze(1).to_broadcast(B3)
                        tt("dve", OH1[ob][:], iot, T3[:, t0:t0 + 16].unsqueeze(2).to_broadcast(B3), ALU.is_equal,
                           [dIOTA, dT3], [dOH1[ob]])
                        tt("dve", OH1[ob][:], OH1[ob][:], T3[:, 256 + t0:256 + t0 + 16].unsqueeze(2).to_broadcast(B3), ALU.mult,
                           [dOH1[ob], dT3], [dOH1[ob]])
                        tt("dve", OH2[ob][:], iot, T3[:, 128 + t0:128 + t0 + 16].unsqueeze(2).to_broadcast(B3), ALU.is_equal,
                           [dIOTA, dT3], [dOH2[ob]])
                        for q4 in range(4):
                            gb = 5 if (gcn[0] % 2 == 0) else 7
                            gcn[0] += 1
                            for j in range(4):
                                tl = q4 * 4 + j
                                mm(PB_[gb][:, j * 128:(j + 1) * 128], OH1[ob][:, tl, :], OH2[ob][:, tl, :], True, True,
                                   [dOH1[ob], dOH2[ob]], [dB[gb]])
                            tg = t2 * 128 + t0 + q4 * 4
                            cp("act", GG[:, :, tg:tg + 4].rearrange("p i t -> p t i"),
                               PB_[gb][:].rearrange("p (t i) -> p t i", i=128), [dB[gb]], [dGG])

            def dense(c):
                cb = c % 2

                def stage1(i2):
                    gi = i2 // 2; bl = i2 % 2
                    if bl == 0:
                        ub = ugn[0] % NUB; ugn[0] += 1
                        ld(UG[ub][:].rearrange("p k n -> p (k n)"), uTb_r[gi], [dUG[ub]])
                        ld(VG[ub][:], vRb[gi * 256:(gi + 1) * 256, :].rearrange("(b p) n -> p b n", p=128), [dVG[ub]])
                    ub = (ugn[0] - 1) % NUB
                    r = (i2 % 2) * 256
                    for k in range(8):
                        mm(PB_[4][:, r:r + 256], UG[ub][:, k, bl * 128:(bl + 1) * 128], H2C[cb][:, k, :], k == 0, k == 7,
                           [dUG[ub], dH2C[cb]], [dB4h[i2 % 2]])
                    return ub
                ubs = {}
                ubs[0] = stage1(0)
                for i2 in range(128):
                    if i2 + 1 < 128:
                        ubs[i2 + 1] = stage1(i2 + 1)
                    bl = i2 % 2; ub = ubs[i2]
                    r = (i2 % 2) * 256
                    act(GE[i2 % 2][:], PB_[4][:, r:r + 256], ACTF.Gelu_apprx_tanh, [dB4h[i2 % 2]], [dGE[i2 % 2]])
                    tt("pool", WT[i2 % 2][:], GE[i2 % 2][:], GG[:, i2, :], ALU.mult, [dGE[i2 % 2], dGG], [dWT[i2 % 2]])
                    for t2 in range(2):
                        for hf in range(2):
                            bk = t2 * 2 + hf
                            mm(PB_[bk][:], WT[i2 % 2][:, t2 * 128:(t2 + 1) * 128], VG[ub][:, bl, hf * 512:(hf + 1) * 512],
                               i2 == 0, i2 == 127, [dWT[i2 % 2], dVG[ub]], [dB[bk]])

            def epilogue(c):
                for t2 in range(2):
                    ti = c * 2 + t2
                    ld(X1t[:], x1d[ti * 128:(ti + 1) * 128, :], [dX1t])
                    for hf in range(2):
                        tt("dve", TMP[:, hf * 512:(hf + 1) * 512], PB_[t2 * 2 + hf][:], BC7[:, 0, hf * 512:(hf + 1) * 512], ALU.mult,
                           [dB[t2 * 2 + hf], dBC7], [dTMP])
                    stt("dve", TMP[:], X1t[:], ALPHA, TMP[:], ALU.mult, ALU.add, [dX1t, dTMP], [dTMP])
                    ln7(TMP, dTMP, OUTt, dOUTt)
                    st_(out[ti * 128:(ti + 1) * 128, :], OUTt[:], [dOUTt])

            stageA_front(0); stageA_back(0); stageB(0)
            for c in range(NCH):
                if c + 1 < NCH:
                    stageA_front(c + 1)
                dense(c)
                epilogue(c)
                if c + 1 < NCH:
                    stageA_back(c + 1)
                    stageB(c + 1)
            fw.finish()
            end_phase()
    return nc


def _prep_shared(inp):
    f = np.float32
    w_in = np.asarray(inp["w_in"][0], f)
    perm64 = np.concatenate([np.arange(32, 64), np.arange(0, 32)])
    perm128 = np.concatenate([perm64, 64 + perm64])
    Wq = w_in[:, 0:1024]; Wk = w_in[:, 3328:4352]; Wv = w_in[:, 4352:5376]
    w_da = np.stack([np.concatenate([Wq[:, h * 128:(h + 1) * 128], Wq[:, h * 128 + perm128],
                                     Wk[:, h * 128:(h + 1) * 128], Wk[:, h * 128 + perm128],
                                     Wv[:, h * 128:(h + 1) * 128]], axis=1) for h in range(8)])
    w_lat = np.concatenate([w_in[:, 1024:1280], w_in[:, 5376:5632], w_in[:, 5632:5696], w_in[:, 5632 + perm64]], axis=1)
    wq = np.asarray(inp["w_q_up"][0], f); wkv = np.asarray(inp["w_kv_up"][0], f)
    w_mla = np.stack([np.concatenate([wq[:, h * 192:h * 192 + 128], wq[:, h * 192 + 128:h * 192 + 192],
                                      wq[:, h * 192 + 128 + perm64], wkv[:, h * 256:h * 256 + 128],
                                      wkv[:, h * 256 + 128:h * 256 + 256]], axis=1) for h in range(8)])
    rows4 = np.concatenate([inp["ln1_g"][0], inp["ln1_b"][0], inp["ln2_g"][0], inp["ln2_b"][0]]).astype(f)[None]
    lamrow = np.concatenate([inp["lambda_q1"][0], inp["lambda_k1"][0], inp["lambda_q2"][0], inp["lambda_k2"][0]]).astype(f)[None]
    colsm = np.concatenate([np.asarray(inp["q_norm_g"][0], f).reshape(2, 128).T, np.asarray(inp["kv_norm_g"][0], f).reshape(2, 128).T,
                            np.asarray(inp["subln_g"][0], f).reshape(128, 1)], axis=1)
    quarter = 16
    inv = (10000.0 ** (-np.arange(quarter, dtype=f) / quarter)).astype(f)
    tpos = np.arange(T)
    ang = np.concatenate([(tpos // 64).astype(f)[:, None] * inv, (tpos % 64).astype(f)[:, None] * inv], axis=1).astype(f)
    p = np.arange(128)
    cosT = np.cos(ang).astype(f).T[p % 32]
    sinT = np.sin(ang).astype(f).T[p % 32]
    sign = np.where((p % 64) < 32, -1.0, 1.0).astype(f)[:, None]
    tabC = np.concatenate([np.ones((128, NCTX), f), cosT], axis=1)
    tabS = np.concatenate([np.zeros((128, NCTX), f), sinT * sign], axis=1)
    b_mod = np.asarray(inp["b_mod"][0], f)
    keys = np.asarray(inp["peer_keys"][0], f)
    keysT = np.ascontiguousarray(keys.transpose(3, 0, 1, 2).reshape(128, 16 * 128))
    U = np.asarray(inp["peer_u"][0], f); V = np.asarray(inp["peer_v"][0], f)
    uT0 = U.reshape(128, 128, 1024).transpose(2, 1, 0).reshape(8, 128, 64, 256)
    uT = np.ascontiguousarray(uT0.transpose(2, 1, 0, 3).reshape(64, 128, 2048))
    vR = np.ascontiguousarray(V.reshape(128, 128, 1024).transpose(1, 0, 2).reshape(16384, 1024))
    sh = {
        "w_mod": np.ascontiguousarray(inp["w_mod"][0], f), "b_row": b_mod[None].copy(),
        "b_col": np.ascontiguousarray(b_mod.reshape(48, 128).T),
        "w_lat": np.ascontiguousarray(w_lat), "w_da": np.ascontiguousarray(w_da), "w_mla": np.ascontiguousarray(w_mla),
        "w_gate": np.ascontiguousarray(w_in[:, 1280:3328]),
        "w_pa": np.ascontiguousarray(inp["w_pa"][0], f), "w_pb": np.ascontiguousarray(inp["w_pb"][0], f),
        "w_out": np.ascontiguousarray(inp["w_out"][0], f),
        "rows4": rows4, "lamrow": lamrow, "colsm": np.ascontiguousarray(colsm),
        "tabC": np.ascontiguousarray(tabC), "tabS": np.ascontiguousarray(tabS),
        "ident": np.eye(128, dtype=f), "iota": np.ascontiguousarray(np.tile(np.arange(128, dtype=f)[None], (128, 1))),
        "w_pq": np.ascontiguousarray(inp["w_pq"][0], f), "keysT": keysT, "uT": uT, "vR": vR,
    }
    return sh


def _in_maps(inp):
    sh = _prep_shared(inp)
    f = np.float32
    cctx = np.asarray(inp["c_ctx"], f).reshape(8, 128).T
    maps = []
    for b in range(8):
        m = dict(sh)
        m["x"] = np.ascontiguousarray(inp["x"][b], f)
        m["ctx"] = np.ascontiguousarray(inp["ctx"][b], f)
        m["cT"] = np.ascontiguousarray(np.concatenate([np.asarray(inp["c"][b], f).reshape(8, 128).T, cctx], axis=1))
        maps.append(m)
    return maps


def kernel(**inputs):
    inp = {k: np.asarray(v) for k, v in inputs.items()}
    nc = build_program()
    res = run_bass_kernel_spmd(nc, _in_maps(inp), core_ids=list(range(8)))
    return np.stack([np.asarray(r["out"], np.float32) for r in res.results], axis=0)
```

```python
import numpy as np
import concourse.bass as bass
import concourse.mybir as mybir

F32 = mybir.dt.float32
BF16 = mybir.dt.bfloat16
U32 = mybir.dt.uint32
ALU = mybir.AluOpType
ACTF = mybir.ActivationFunctionType
AX = mybir.AxisListType


class Dep:
    __slots__ = ("w", "r")

    def __init__(self):
        self.w = {}
        self.r = {}


class FW:
    NDMA = 24
    SELF_SYNC = True
    NOSELF = ("act",)

    def __init__(self, nc, stack):
        self.nc = nc
        self.engs = {"pe": nc.tensor, "act": nc.scalar, "dve": nc.vector,
                     "pool": nc.gpsimd, "sp": nc.sync}
        self.sems = {}
        self.count = {}
        self.known = {}
        self.hist = {}
        self.prog = {k: [] for k in self.engs}
        for k in self.engs:
            self.sems[k] = stack.enter_context(nc.semaphore("s_" + k))
            self.count[k] = 0
            self.known[k] = {}
            self.hist[k] = {}
        self.dma_pool = {}
        for q in ("sp", "pool", "act"):
            lst = []
            for i in range(self.NDMA):
                key = "d_%s_%d" % (q, i)
                self.sems[key] = stack.enter_context(nc.semaphore(key))
                self.count[key] = 0
                self.hist[key] = {}
                lst.append(key)
            self.dma_pool[q] = [lst, 0]

    def _learn(self, e, key, val):
        kn = self.known[e]
        if kn.get(key, 0) < val:
            kn[key] = val
        snap = self.hist[key].get(val)
        if snap:
            for k2, v2 in snap.items():
                if kn.get(k2, 0) < v2:
                    kn[k2] = v2

    def _waits(self, e, reads, writes, extra=()):
        need = {}
        for d in reads:
            for k, v in d.w.items():
                if need.get(k, 0) < v:
                    need[k] = v
        for d in writes:
            for k, v in d.w.items():
                if need.get(k, 0) < v:
                    need[k] = v
            for k, v in d.r.items():
                if need.get(k, 0) < v:
                    need[k] = v
        for k, v in extra:
            if need.get(k, 0) < v:
                need[k] = v
        kn = self.known[e]
        for k, v in need.items():
            if k == e and (e == "pe" or e in self.NOSELF):
                continue
            if kn.get(k, 0) >= v:
                continue
            eng = self.engs[e]
            sem = self.sems[k]
            self.prog[e].append((lambda eng=eng, sem=sem, v=v: eng.wait_ge(sem, v)))
            self._learn(e, k, v)

    def op(self, e, fn, reads=(), writes=()):
        self._waits(e, reads, writes)
        self.count[e] += 1
        c = self.count[e]
        eng = self.engs[e]
        sem = self.sems[e]
        self.prog[e].append((lambda eng=eng, sem=sem, fn=fn: fn(eng).then_inc(sem, 1)))
        self.hist[e][c] = dict(self.known[e])
        for d in reads:
            d.r[e] = c
        for d in writes:
            d.w[e] = c

    def dma(self, q, out, in_, reads=(), writes=(), **kw):
        lst, idx = self.dma_pool[q]
        key = lst[idx % len(lst)]
        self.dma_pool[q][1] = idx + 1
        prev = self.count[key]
        extra = [(key, prev)] if prev > 0 else []
        self._waits(q, reads, writes, extra)
        self.count[key] = prev + 16
        v = prev + 16
        eng = self.engs[q]
        sem = self.sems[key]
        self.prog[q].append((lambda eng=eng, sem=sem, out=out, in_=in_, kw=kw:
                             eng.dma_start(out=out, in_=in_, **kw).then_inc(sem, 16)))
        self.hist[key][v] = dict(self.known[q])
        for d in reads:
            d.r[key] = v
        for d in writes:
            d.w[key] = v

    def barrier(self):
        for e in self.engs:
            for k, v in self.count.items():
                if k == e or v == 0:
                    continue
                if self.known[e].get(k, 0) < v:
                    eng = self.engs[e]
                    sem = self.sems[k]
                    self.prog[e].append((lambda eng=eng, sem=sem, v=v: eng.wait_ge(sem, v)))
                    self.known[e][k] = v

    def finish(self):
        for q in ("sp", "pool", "act"):
            lst, idx = self.dma_pool[q]
            for key in lst:
                v = self.count[key]
                if v > 0 and self.known[q].get(key, 0) < v:
                    eng = self.engs[q]
                    sem = self.sems[key]
                    self.prog[q].append((lambda eng=eng, sem=sem, v=v: eng.wait_ge(sem, v)))
                    self.known[q][key] = v

    def emit(self, block):
        prog = self.prog

        @block.tensor
        def _(e):
            for f in prog["pe"]:
                f()

        @block.scalar
        def _(e):
            for f in prog["act"]:
                f()

        @block.vector
        def _(e):
            for f in prog["dve"]:
                f()

        @block.gpsimd
        def _(e):
            for f in prog["pool"]:
                f()

        @block.sync
        def _(e):
            for f in prog["sp"]:
                f()


from contextlib import ExitStack
from concourse.bass_utils import run_bass_kernel_spmd

T = 4096
NCTX = 256
NK = T + NCTX
NKT = NK // 128
D = 1024
ALPHA = 2.0 ** 0.25
EPS = 1e-6
LAMBDA_INIT = 0.2
NEG = -1.0e30


def build_program(skip=(), dbg=False, NCH=16):
    nc = bass.Bass("TRN2", target_bir_lowering=False)

    def din(name, shape, dt=F32):
        return nc.dram_tensor(name, shape, dt, kind="ExternalInput").ap()

    def dscr(name, shape, dt):
        kind = "ExternalOutput" if dbg else "Internal"
        return nc.dram_tensor(name, shape, dt, kind=kind).ap()

    x = din("x", [T, D]); ctx = din("ctx", [NCTX, D]); cT = din("cT", [128, 16])
    w_mod = din("w_mod", [D, 6144]); b_row = din("b_row", [1, 6144]); b_col = din("b_col", [128, 48])
    w_lat = din("w_lat", [D, 640]); w_da = din("w_da", [8, D, 640]); w_mla = din("w_mla", [8, 256, 512])
    w_gate = din("w_gate", [D, 2048]); w_pa = din("w_pa", [D, D]); w_pb = din("w_pb", [D, D]); w_out = din("w_out", [D, D])
    rows4 = din("rows4", [1, 4096]); lamrow = din("lamrow", [1, 256]); colsm = din("colsm", [128, 5])
    tabC = din("tabC", [128, NK]); tabS = din("tabS", [128, NK])
    ident_d = din("ident", [128, 128]); iota_d = din("iota", [128, 128])
    w_pq = din("w_pq", [D, 2048]); keysT = din("keysT", [128, 16 * 128])
    uT = din("uT", [64, 128, 2048]); vR = din("vR", [16384, D])
    out = nc.dram_tensor("out", [T, D], F32, kind="ExternalOutput").ap()

    yad = dscr("yad", [8, 128, T], BF16); ybd = dscr("ybd", [8, 128, T], BF16)
    x1d = dscr("x1d", [T, D], F32); h2Td = dscr("h2Td", [128, 8, T], BF16)
    uTb_r = nc.dram_tensor("uTb_r", [64, 128, 2048], BF16).ap(); vRb = nc.dram_tensor("vRb", [16384, D], BF16).ap()
    wpqb = nc.dram_tensor("wpqb", [D, 2048], BF16).ap()
    zTd = nc.dram_tensor("zTd", [128, 8, T], BF16).ap()
    bctd = nc.dram_tensor("bctd", [128, 8, 1024], F32).ap()

    with ExitStack() as gst:
        fw = FW(nc, gst)

        def sbt(st, name, shape, dt):
            return st.enter_context(nc.sbuf_tensor(name, shape, dt))

        PB_ = [gst.enter_context(nc.psum_tensor("bank%d" % i, [128, 512], F32)) for i in range(8)]
        dB = [Dep() for _ in range(8)]

        def mm(o, l, r, start, stop, reads, writes):
            fw.op("pe", lambda e: e.matmul(o, l, r, start=start, stop=stop), reads, writes)

        def tr(o, i, idn, reads, writes):
            fw.op("pe", lambda e: e.transpose(o, i, idn), reads, writes)

        def act(o, i, func, reads, writes, bias=0.0, scale=1.0, accum=None):
            if accum is None:
                fw.op("act", lambda e: e.activation(o, i, func, bias=bias, scale=scale), reads, writes)
            else:
                fw.op("act", lambda e: e.activation(o, i, func, bias=bias, scale=scale, accum_out=accum), reads, writes)

        def tt(eng, o, a, b, op, reads, writes):
            fw.op(eng, lambda e: e.tensor_tensor(o, a, b, op), reads, writes)

        def ts(eng, o, a, s1, s2, op0, op1, reads, writes):
            if s2 is None:
                fw.op(eng, lambda e: e.tensor_scalar(o, a, s1, None, op0), reads, writes)
            else:
                fw.op(eng, lambda e: e.tensor_scalar(o, a, s1, s2, op0, op1), reads, writes)

        def end_phase():
            fw.barrier()
            with nc.Block() as blk:
                fw.emit(blk)
            for k in fw.prog:
                fw.prog[k] = []

        def stt(eng, o, a, s, b, op0, op1, reads, writes):
            fw.op(eng, lambda e: e.scalar_tensor_tensor(o, a, s, b, op0, op1), reads, writes)

        def cp(eng, o, i, reads, writes):
            if eng == "act":
                fw.op("act", lambda e: e.copy(o, i), reads, writes)
            else:
                fw.op(eng, lambda e: e.tensor_copy(o, i), reads, writes)

        def recip(o, i, reads, writes):
            fw.op("dve", lambda e: e.reciprocal(o, i), reads, writes)

        def ld(o, i, writes, reads=()):
            fw.dma("sp", o, i, reads=reads, writes=writes)

        def st_(o, i, reads, writes=()):
            fw.dma("sp", o, i, reads=reads, writes=writes)

        IDENT = sbt(gst, "IDENT", [128, 128], F32); dIDENT = Dep()
        IOTA = sbt(gst, "IOTA", [128, 128], F32); dIOTA = Dep()
        ONES32 = sbt(gst, "ONES32", [128, 128], F32); dONES32 = Dep()
        ONES16 = sbt(gst, "ONES16", [128, 128], BF16); dONES16 = Dep()
        MODT = sbt(gst, "MODT", [128, 48, 2], F32); dMODT = Dep()
        SC1P = sbt(gst, "SC1P", [128, 8, 2], F32); dSC1P = Dep()
        NLAM = sbt(gst, "NLAM", [128, 1], F32); dNLAM = Dep()
        COLS = sbt(gst, "COLS", [128, 5], F32); dCOLS = Dep()
        SGC = sbt(gst, "SGC", [128, 1], F32); dSGC = Dep()
        ld(IDENT[:], ident_d, [dIDENT]); ld(IOTA[:], iota_d, [dIOTA]); ld(COLS[:], colsm, [dCOLS])
        fw.op("dve", lambda e: e.memset(ONES32[:], 1.0), (), [dONES32])
        fw.op("dve", lambda e: e.memset(ONES16[:], 1.0), (), [dONES16])
        fw.op("dve", lambda e: e.tensor_scalar(SGC[:], COLS[:, 4:5], 1.0 - LAMBDA_INIT, None, ALU.mult), [dCOLS], [dSGC])

        with ExitStack() as st:
            BCT = sbt(st, "BCT", [128, 8, 1024], F32); dBCT = Dep()
            CT = sbt(st, "CT", [128, 16], F32); dCT = Dep()
            SL = sbt(st, "SL", [128, 8, 2], F32); dSL = Dep()
            BCOL = sbt(st, "BCOL", [128, 48], F32); dBCOL = Dep()
            MODR = sbt(st, "MODR", [2, 6144], F32); dMODR = Dep()
            BROW = sbt(st, "BROW", [2, 6144], F32); dBROW = Dep()
            ROWS = sbt(st, "ROWS", [1, 4096], F32); dROWS = Dep()
            LAMR = sbt(st, "LAMR", [1, 256], F32); dLAMR = Dep()
            LAMB = sbt(st, "LAMB", [128, 256], F32); dLAMB = Dep()
            LTMP = sbt(st, "LTMP", [128, 4], F32); dLTMP = Dep()
            WM = [sbt(st, "WM%d" % i, [128, 8, 512], F32) for i in range(2)]; dWM = [Dep(), Dep()]
            ld(CT[:], cT, [dCT]); ld(BCOL[:], b_col, [dBCOL])
            ld(BROW[0:1, :], b_row, [dBROW]); ld(BROW[1:2, :], b_row, [dBROW])
            ld(ROWS[:], rows4, [dROWS]); ld(LAMR[:], lamrow, [dLAMR])
            act(SL[:, :, 0], CT[:, 0:8], ACTF.Silu, [dCT], [dSL])
            act(SL[:, :, 1], CT[:, 8:16], ACTF.Silu, [dCT], [dSL])
            wmv = w_mod.rearrange("(k p) n -> p k n", p=128)
            for j in range(12):
                W_ = WM[j % 2]; dW_ = dWM[j % 2]
                ld(W_[:], wmv[:, :, j * 512:(j + 1) * 512], [dW_])
                for cc in range(4):
                    for k in range(8):
                        mm(PB_[0][:, cc * 2:cc * 2 + 2], W_[:, k, cc * 128:(cc + 1) * 128], SL[:, k, :],
                           k == 0, k == 7, [dW_, dSL], [dB[0]])
                tt("dve", MODT[:, j * 4:(j + 1) * 4, :], PB_[0][:, 0:8].rearrange("p (c j) -> p c j", j=2),
                   BCOL[:, j * 4:(j + 1) * 4].unsqueeze(2).to_broadcast([128, 4, 2]), ALU.add, [dB[0], dBCOL], [dMODT])
                for k in range(8):
                    mm(PB_[1][0:2, :], SL[:, k, :], W_[:, k, :], k == 0, k == 7, [dW_, dSL], [dB[1]])
                tt("dve", MODR[:, j * 512:(j + 1) * 512], PB_[1][0:2, :], BROW[:, j * 512:(j + 1) * 512], ALU.add,
                   [dB[1], dBROW], [dMODR])
            ts("dve", SC1P[:], MODT[:, 8:16, :], 1.0, None, ALU.add, ALU.bypass, [dMODT], [dSC1P])
            srcs = [(MODR, 2048, dMODR), (MODR, 3072, dMODR), (MODR, 4096, dMODR), (MODR, 5120, dMODR),
                    (ROWS, 0, dROWS), (ROWS, 1024, dROWS), (ROWS, 2048, dROWS), (ROWS, 3072, dROWS)]
            for i, (src, off, dsrc) in enumerate(srcs):
                for hf in range(2):
                    bk = 2 + hf
                    mm(PB_[bk][:], ONES32[0:1, :], src[0:1, off + hf * 512: off + (hf + 1) * 512], True, True,
                       [dONES32, dsrc], [dB[bk]])
                    if i == 2:
                        ts("dve", BCT[:, i, hf * 512:(hf + 1) * 512], PB_[bk][:], 1.0, None, ALU.add, ALU.bypass,
                           [dB[bk]], [dBCT])
                    else:
                        cp("dve", BCT[:, i, hf * 512:(hf + 1) * 512], PB_[bk][:], [dB[bk]], [dBCT])
            st_(bctd, BCT[:], [dBCT])
            mm(PB_[4][:, 0:256], ONES32[0:1, :], LAMR[0:1, :], True, True, [dONES32, dLAMR], [dB[4]])
            cp("dve", LAMB[:], PB_[4][:, 0:256], [dB[4]], [dLAMB])
            tt("dve", LAMB[:, 0:64], LAMB[:, 0:64], LAMB[:, 64:128], ALU.mult, [dLAMB], [dLAMB])
            tt("dve", LAMB[:, 128:192], LAMB[:, 128:192], LAMB[:, 192:256], ALU.mult, [dLAMB], [dLAMB])
            fw.op("dve", lambda e: e.reduce_sum(LTMP[:, 0:1], LAMB[:, 0:64], AX.X), [dLAMB], [dLTMP])
            fw.op("dve", lambda e: e.reduce_sum(LTMP[:, 1:2], LAMB[:, 128:192], AX.X), [dLAMB], [dLTMP])
            act(LTMP[:, 2:4], LTMP[:, 0:2], ACTF.Exp, [dLTMP], [dLTMP])
            tt("dve", NLAM[:], LTMP[:, 3:4], LTMP[:, 2:3], ALU.subtract, [dLTMP], [dNLAM])
            ts("dve", NLAM[:], NLAM[:], -LAMBDA_INIT, None, ALU.add, ALU.bypass, [dNLAM], [dNLAM])
            end_phase()

        with ExitStack() as ast:
            HK = sbt(ast, "HK", [128, 8, NK], BF16); dHK = Dep()
            with ExitStack() as st:
                XT = [sbt(st, "XT%d" % i, [128, D], F32) for i in range(2)]; dXT = [Dep(), Dep()]
                for i in range(NKT):
                    X_ = XT[i % 2]; dX_ = dXT[i % 2]
                    src = ctx[i * 128:(i + 1) * 128, :] if i < 2 else x[(i - 2) * 128:(i - 1) * 128, :]
                    j = 1 if i < 2 else 0
                    ld(X_[:], src, [dX_])
                    for k in range(8):
                        bk = k // 4
                        tr(PB_[bk][:, (k % 4) * 128:(k % 4 + 1) * 128], X_[:, k * 128:(k + 1) * 128], IDENT[:],
                           [dX_, dIDENT], [dB[bk]])
                    for k in range(8):
                        bk = k // 4
                        ts("dve", HK[:, k, i * 128:(i + 1) * 128], PB_[bk][:, (k % 4) * 128:(k % 4 + 1) * 128],
                           SC1P[:, k, j:j + 1], MODT[:, k, j:j + 1], ALU.mult, ALU.add, [dB[bk], dSC1P, dMODT], [dHK])
                end_phase()

            with ExitStack() as rst:
                TCc = [sbt(rst, "TCc%d" % i, [128, 512], F32) for i in range(2)]; dTCc = [Dep(), Dep()]
                TSc = [sbt(rst, "TSc%d" % i, [128, 512], F32) for i in range(2)]; dTSc = [Dep(), Dep()]
                tabn = [0]

                def load_tab(np_, to, sl):
                    i = tabn[0] % 2; tabn[0] += 1
                    ld(TCc[i][0:np_, 0:sl], tabC[0:np_, to:to + sl], [dTCc[i]])
                    ld(TSc[i][0:np_, 0:sl], tabS[0:np_, to:to + sl], [dTSc[i]])
                    return TCc[i], dTCc[i], TSc[i], dTSc[i]
                QNT = sbt(rst, "QNT", [128, 2, T], BF16); dQNT = Dep()
                KVNT = sbt(rst, "KVNT", [128, 2, NK], BF16); dKVNT = Dep()
                KR = sbt(rst, "KR", [128, NK], BF16); dKR = Dep()
                fw.op("pool", lambda e: e.memset(KR[64:128, :], 0.0), (), [dKR])
                chunks_k = [(c * 512, 512) for c in range(8)] + [(4096, 256)]
                with ExitStack() as st:
                    WL32 = sbt(st, "WL32", [128, 8, 640], F32); dWL32 = Dep()
                    WL = sbt(st, "WL", [128, 8, 640], BF16); dWL = Dep()
                    SQ = sbt(st, "SQ", [128, 2, 512], F32); dSQ = Dep()
                    RS = sbt(st, "RS", [128, 512], F32); dRS = Dep()
                    T1 = sbt(st, "T1", [64, 512], F32); dT1 = Dep()
                    T2 = sbt(st, "T2", [64, 512], F32); dT2 = Dep()
                    ld(WL32[:], w_lat.rearrange("(k p) n -> p k n", p=128), [dWL32])
                    cp("act", WL[:], WL32[:], [dWL32], [dWL])

                    def latent(colbase, gcol, dst, ddst, koff, klen, dcol):
                        for m in range(2):
                            for k in range(8):
                                mm(PB_[m][:, 0:klen], WL[:, k, colbase + m * 128: colbase + (m + 1) * 128],
                                   HK[:, k, koff:koff + klen], k == 0, k == 7, [dWL, dHK], [dB[m]])
                            act(SQ[:, m, 0:klen], PB_[m][:, 0:klen], ACTF.Square, [dB[m]], [dSQ])
                        for m in range(2):
                            mm(PB_[2][:, 0:klen], ONES32[:], SQ[:, m, 0:klen], m == 0, m == 1, [dONES32, dSQ], [dB[2]])
                        act(RS[:, 0:klen], PB_[2][:, 0:klen], ACTF.Sqrt, [dB[2]], [dRS], bias=EPS, scale=1.0 / 256.0)
                        recip(RS[:, 0:klen], RS[:, 0:klen], [dRS], [dRS])
                        for m in range(2):
                            stt("dve", dst[:, m, dcol:dcol + klen], PB_[m][:, 0:klen], COLS[:, gcol + m:gcol + m + 1],
                                RS[:, 0:klen], ALU.mult, ALU.mult, [dB[m], dCOLS, dRS], [ddst])

                    for (ko, kl) in chunks_k:
                        latent(256, 2, KVNT, dKVNT, ko, kl, ko)
                        for k in range(8):
                            mm(PB_[3][0:64, 0:kl], WL[:, k, 512:576], HK[:, k, ko:ko + kl], k == 0, k == 7, [dWL, dHK], [dB[3]])
                        for k in range(8):
                            mm(PB_[4][0:64, 0:kl], WL[:, k, 576:640], HK[:, k, ko:ko + kl], k == 0, k == 7, [dWL, dHK], [dB[4]])
                        TC_, dTC_, TS_, dTS_ = load_tab(64, ko, kl)
                        tt("dve", T1[:, 0:kl], PB_[3][0:64, 0:kl], TC_[0:64, 0:kl], ALU.mult, [dB[3], dTC_], [dT1])
                        tt("dve", T2[:, 0:kl], PB_[4][0:64, 0:kl], TS_[0:64, 0:kl], ALU.mult, [dB[4], dTS_], [dT2])
                        tt("pool", KR[0:64, ko:ko + kl], T1[:, 0:kl], T2[:, 0:kl], ALU.add, [dT1, dT2], [dKR])
                    for c in range(8):
                        latent(0, 0, QNT, dQNT, NCTX + c * 512, 512, c * 512)
                    end_phase()
                with ExitStack() as st:
                    PT = [sbt(st, "PT%d" % i, [128, 512], BF16) for i in range(3)]; dPT = [Dep() for _ in range(3)]
                    RZ = sbt(st, "RZ", [128, 512], F32); dRZ = Dep()
                    ZA = sbt(st, "ZA", [128, 512], F32); dZA = Dep()
                    T1 = [sbt(st, "T1_%d" % i, [128, 512], F32) for i in range(1)]; dT1 = [Dep(), Dep()]
                    T2 = [sbt(st, "T2_%d" % i, [128, 512], F32) for i in range(1)]; dT2 = [Dep(), Dep()]
                    QH = [sbt(st, "QH%d" % i, [128, T], BF16) for i in range(1)]; dQH = [Dep(), Dep()]
                    KH = [sbt(st, "KH%d" % i, [128, NK], BF16) for i in range(1)]; dKH = [Dep(), Dep()]
                    VH = [sbt(st, "VH%d" % i, [128, NKT, 128], BF16) for i in range(1)]; dVH = [Dep(), Dep()]
                    AUX = sbt(st, "AUX", [128, NK], BF16); dAUX = Dep()
                    QRH = [AUX]; dQRH = [dAUX, dAUX]
                    WS = [sbt(st, "WS%d" % i, [128, 8, 128], F32) for i in range(1)]; dWS = [Dep(), Dep()]
                    WH = [sbt(st, "WH%d" % i, [128, 8, 640], BF16) for i in range(1)]; dWH = [Dep(), Dep()]
                    OM = [sbt(st, "OM%d" % i, [128, 512], F32) for i in range(2)]; dOM = [Dep(), Dep()]
                    DH = sbt(st, "DH", [128, 512], F32); dDH = Dep()
                    SQ2 = sbt(st, "SQ2", [128, 512], F32); dSQ2 = Dep()
                    RS2 = sbt(st, "RS2", [128, 512], F32); dRS2 = Dep()
                    YO = [sbt(st, "YO%d" % i, [128, 512], BF16) for i in range(2)]; dYO = [Dep(), Dep()]
                    tcount = [0]
                    SBK = [0, 1, 2, 7]

                    def attn(Sfn, Vt, dV, scale, si, consume, zdve=False):
                        PO = PB_[3 + 2 * si]; dPO = dB[3 + 2 * si]; PZ = PB_[4 + 2 * si]; dPZ = dB[4 + 2 * si]
                        Sfn(0)
                        Sfn(1)
                        for kt in range(NKT):
                            if kt + 2 < NKT:
                                Sfn(kt + 2)
                            b = kt % 3
                            act(PT[b][:], PB_[SBK[kt % 4]][:], ACTF.Exp, [dB[SBK[kt % 4]]], [dPT[b]], scale=scale)
                            mm(PO[:], Vt[:, kt, :], PT[b][:], kt == 0, kt == NKT - 1, [dV, dPT[b]], [dPO])
                            if not zdve:
                                mm(PZ[:], ONES16[:], PT[b][:], kt == 0, kt == NKT - 1, [dONES16, dPT[b]], [dPZ])
                            elif kt == 0:
                                cp("dve", ZA[:], PT[b][:], [dPT[b]], [dZA])
                            else:
                                tt("dve", ZA[:], ZA[:], PT[b][:], ALU.add, [dZA, dPT[b]], [dZA])
                        if zdve:
                            mm(PZ[:], ONES32[:], ZA[:], True, True, [dONES32, dZA], [dPZ])
                        recip(RZ[:], PZ[:], [dPZ], [dRZ])
                        consume(PO, dPO)

                    def rope_proj(W_, dW_, ca, cb, src, dsrc, so, sl, to, dst, ddst, do, np_, dst2=None, ddst2=None):
                        nk_ = src.shape[1]
                        for k in range(nk_):
                            mm(PB_[0][0:np_, 0:sl], W_[:, k, ca:ca + np_], src[:, k, so:so + sl], k == 0, k == nk_ - 1,
                               [dW_, dsrc], [dB[0]])
                        for k in range(nk_):
                            mm(PB_[1][0:np_, 0:sl], W_[:, k, cb:cb + np_], src[:, k, so:so + sl], k == 0, k == nk_ - 1,
                               [dW_, dsrc], [dB[1]])
                        i = 0
                        TC_, dTC_, TS_, dTS_ = load_tab(np_, to, sl)
                        tt("dve", T1[i][0:np_, 0:sl], PB_[0][0:np_, 0:sl], TC_[0:np_, 0:sl], ALU.mult, [dB[0], dTC_], [dT1[i]])
                        tt("dve", T2[i][0:np_, 0:sl], PB_[1][0:np_, 0:sl], TS_[0:np_, 0:sl], ALU.mult, [dB[1], dTS_], [dT2[i]])
                        if dst2 is None:
                            tt("pool", dst[0:np_, do:do + sl], T1[i][0:np_, 0:sl], T2[i][0:np_, 0:sl], ALU.add, [dT1[i], dT2[i]], [ddst])
                        else:
                            tt("pool", dst[0:64, do:do + sl], T1[i][0:64, 0:sl], T2[i][0:64, 0:sl], ALU.add, [dT1[i], dT2[i]], [ddst])
                            tt("pool", dst2[64:128, do:do + sl], T1[i][64:128, 0:sl], T2[i][64:128, 0:sl], ALU.add, [dT1[i], dT2[i]], [ddst2])

                    def vproj(W_, dW_, c0, src, dsrc, Vt, dVt):
                        nk_ = src.shape[1]
                        for g0 in range(0, NKT, 4):
                            n = min(4, NKT - g0)
                            for j in range(n):
                                kt = g0 + j
                                for k in range(nk_):
                                    mm(PB_[7][:, j * 128:(j + 1) * 128], src[:, k, kt * 128:(kt + 1) * 128], W_[:, k, c0:c0 + 128],
                                       k == 0, k == nk_ - 1, [dsrc, dW_], [dB[7]])
                            cp("act", Vt[:, g0:g0 + n, :], PB_[7][:, 0:n * 128].rearrange("p (a b) -> p a b", b=128), [dB[7]], [dVt])

                    cast_jobs = []
                    for i in range(32):
                        cast_jobs.append((uTb_r[i * 2:(i + 1) * 2], uT[i * 2:(i + 1) * 2]))
                    for i in range(32):
                        cast_jobs.append((vRb[i * 512:(i + 1) * 512, :], vR[i * 512:(i + 1) * 512, :]))
                    for i in range(2):
                        cast_jobs.append((wpqb[i * 512:(i + 1) * 512, :], w_pq[i * 512:(i + 1) * 512, :]))

                    def cast_step(n):
                        for _ in range(n):
                            if cast_jobs:
                                o_, i_ = cast_jobs.pop(0)
                                fw.dma("pool", o_, i_)
                    fw.op("pool", lambda e: e.memset(KH[0][64:128, :], 0.0), (), [dKH[0]])
                    fw.op("pool", lambda e: e.memset(AUX[0:64, :], 0.0), (), [dAUX])
                    if "diff" not in skip:
                      for h in range(8):
                        hb = 0
                        cast_step(5)
                        wv = w_da[h].rearrange("(k p) n -> p k n", p=128)
                        for pc in range(5):
                            ld(WS[0][:], wv[:, :, pc * 128:(pc + 1) * 128], [dWS[0]])
                            cp("dve", WH[hb][:, :, pc * 128:(pc + 1) * 128], WS[0][:], [dWS[0]], [dWH[hb]])
                        for qc in range(8):
                            rope_proj(WH[hb], dWH[hb], 0, 128, HK, dHK, NCTX + qc * 512, 512, NCTX + qc * 512, QH[hb], dQH[hb], qc * 512, 128)
                        for (ko, kl) in chunks_k:
                            rope_proj(WH[hb], dWH[hb], 256, 384, HK, dHK, ko, kl, ko, KH[hb], dKH[hb], ko, 128, AUX, dAUX)
                        vproj(WH[hb], dWH[hb], 512, HK, dHK, VH[hb], dVH[hb])
                        for qc in range(8):
                            for m in range(2):
                                def Sfn(kt, m=m, qc=qc):
                                    Km = KH[hb] if m == 0 else AUX
                                    mm(PB_[SBK[kt % 4]][:], Km[:, kt * 128:(kt + 1) * 128],
                                       QH[hb][:, qc * 512:(qc + 1) * 512], True, True, [dKH[hb], dAUX, dQH[hb]], [dB[SBK[kt % 4]]])

                                def consume(PO, dPO, m=m):
                                    tt("dve", OM[m][:], PO[:], RZ[:], ALU.mult, [dPO, dRZ], [dOM[m]])
                                attn(Sfn, VH[hb], dVH[hb], 0.125, m, consume)
                            stt("dve", DH[:], OM[1][:], NLAM[:, 0:1], OM[0][:], ALU.mult, ALU.add,
                                [dOM[0], dOM[1], dNLAM], [dDH])
                            sl_ = slice(qc * 512, (qc + 1) * 512)
                            act(SQ2[:], DH[:], ACTF.Square, [dDH], [dSQ2])
                            mm(PB_[7][:], ONES32[:], SQ2[:], True, True, [dONES32, dSQ2], [dB[7]])
                            act(RS2[:], PB_[7][:], ACTF.Sqrt, [dB[7]], [dRS2], bias=EPS, scale=1.0 / 128.0)
                            recip(RS2[:], RS2[:], [dRS2], [dRS2])
                            stt("dve", YO[qc % 2][:], DH[:], SGC[:, 0:1], RS2[:], ALU.mult, ALU.mult, [dDH, dSGC, dRS2], [dYO[qc % 2]])
                            st_(yad[h, :, sl_], YO[qc % 2][:], [dYO[qc % 2]])

                    WM32 = [sbt(st, "WM32_%d" % i, [128, 2, 512], F32) for i in range(1)]; dWM32 = [Dep(), Dep()]
                    WMb = [sbt(st, "WMb%d" % i, [128, 2, 512], BF16) for i in range(1)]; dWMb = [Dep(), Dep()]
                    if "mla" not in skip:
                      for h in range(8):
                        hb = 0
                        cast_step(5)
                        ld(WM32[hb][:], w_mla[h].rearrange("(k p) n -> p k n", p=128), [dWM32[hb]])
                        cp("dve", WMb[hb][:], WM32[hb][:], [dWM32[hb]], [dWMb[hb]])
                        for qc in range(8):
                            for kk in range(2):
                                mm(PB_[2][:], WMb[hb][:, kk, 0:128], QNT[:, kk, qc * 512:(qc + 1) * 512], kk == 0, kk == 1,
                                   [dWMb[hb], dQNT], [dB[2]])
                            cp("act", QH[hb][:, qc * 512:(qc + 1) * 512], PB_[2][:], [dB[2]], [dQH[hb]])
                            rope_proj(WMb[hb], dWMb[hb], 128, 192, QNT, dQNT, qc * 512, 512, NCTX + qc * 512, QRH[hb], dQRH[hb], qc * 512, 64)
                        for (ko, kl) in chunks_k:
                            for kk in range(2):
                                mm(PB_[2][:, 0:kl], WMb[hb][:, kk, 256:384], KVNT[:, kk, ko:ko + kl], kk == 0, kk == 1,
                                   [dWMb[hb], dKVNT], [dB[2]])
                            cp("act", KH[hb][:, ko:ko + kl], PB_[2][:, 0:kl], [dB[2]], [dKH[hb]])
                        vproj(WMb[hb], dWMb[hb], 384, KVNT, dKVNT, VH[hb], dVH[hb])
                        for qc in range(8):
                            def Sfn(kt, qc=qc):
                                mm(PB_[SBK[kt % 4]][:], KH[hb][:, kt * 128:(kt + 1) * 128], QH[hb][:, qc * 512:(qc + 1) * 512],
                                   True, False, [dKH[hb], dQH[hb]], [dB[SBK[kt % 4]]])
                                mm(PB_[SBK[kt % 4]][:], KR[:, kt * 128:(kt + 1) * 128], QRH[hb][:, qc * 512:(qc + 1) * 512],
                                   False, True, [dKR, dQRH[hb]], [dB[SBK[kt % 4]]])

                            def consume(PO, dPO, qc=qc):
                                tt("dve", YO[qc % 2][:], PO[:], RZ[:], ALU.mult, [dPO, dRZ], [dYO[qc % 2]])
                                st_(ybd[h, :, qc * 512:(qc + 1) * 512], YO[qc % 2][:], [dYO[qc % 2]])
                            attn(Sfn, VH[hb], dVH[hb], 192.0 ** -0.5, qc % 2, consume, zdve=True)
                    cast_step(100)
                    end_phase()
            with ExitStack() as st:
                WST = [sbt(st, "WST%d" % i, [128, 8, 256], F32) for i in range(2)]; dWST = [Dep(), Dep()]
                WPA = sbt(st, "WPA", [128, 8, D], BF16); dWPA = Dep()
                WPB = sbt(st, "WPB", [128, 8, D], BF16); dWPB = Dep()
                WG = sbt(st, "WG", [128, 8, 2048], BF16); dWG = Dep()
                YA = [sbt(st, "YA%d" % i, [128, 8, 512], BF16) for i in range(2)]; dYA = [Dep(), Dep()]
                YB = [sbt(st, "YB%d" % i, [128, 8, 512], BF16) for i in range(2)]; dYB = [Dep(), Dep()]
                GA = sbt(st, "GA", [128, 512], F32); dGA = Dep()
                GB = sbt(st, "GB", [128, 512], F32); dGB = Dep()
                U1 = sbt(st, "U1", [128, 512], F32); dU1 = Dep()
                U2 = sbt(st, "U2", [128, 512], F32); dU2 = Dep()
                ZT = [sbt(st, "ZT%d" % i, [128, 8, 512], BF16) for i in range(2)]; dZT = [Dep(), Dep()]
                wcnt = [0]

                def load_w(dst, ddst, src, ncols):
                    sv = src.rearrange("(k p) n -> p k n", p=128)
                    for c0 in range(0, ncols, 256):
                        i = wcnt[0] % 2; wcnt[0] += 1
                        ld(WST[i][:], sv[:, :, c0:c0 + 256], [dWST[i]])
                        cp("dve" if (wcnt[0] % 2) else "act", dst[:, :, c0:c0 + 256], WST[i][:], [dWST[i]], [ddst])
                load_w(WPA, dWPA, w_pa, D); load_w(WPB, dWPB, w_pb, D); load_w(WG, dWG, w_gate, 2048)
                for qc in range(8):
                    qb = qc % 2
                    sl_ = slice(qc * 512, (qc + 1) * 512)
                    ld(YA[qb][:], yad[:, :, sl_].rearrange("h p t -> p h t"), [dYA[qb]])
                    ld(YB[qb][:], ybd[:, :, sl_].rearrange("h p t -> p h t"), [dYB[qb]])
                    for oc in range(8):
                        oc_ = slice(oc * 128, (oc + 1) * 128)
                        for hh in range(8):
                            mm(PB_[0][:], WPA[:, hh, oc_], YA[qb][:, hh, :], hh == 0, hh == 7, [dWPA, dYA[qb]], [dB[0]])
                        for hh in range(8):
                            mm(PB_[1][:], WPB[:, hh, oc_], YB[qb][:, hh, :], hh == 0, hh == 7, [dWPB, dYB[qb]], [dB[1]])
                        for k in range(8):
                            mm(PB_[2][:], WG[:, k, oc * 128:(oc + 1) * 128], HK[:, k, NCTX + qc * 512:NCTX + (qc + 1) * 512],
                               k == 0, k == 7, [dWG, dHK], [dB[2]])
                        for k in range(8):
                            mm(PB_[3][:], WG[:, k, 1024 + oc * 128:1024 + (oc + 1) * 128], HK[:, k, NCTX + qc * 512:NCTX + (qc + 1) * 512],
                               k == 0, k == 7, [dWG, dHK], [dB[3]])
                        act(GA[:], PB_[2][:], ACTF.Sigmoid, [dB[2]], [dGA])
                        act(GB[:], PB_[3][:], ACTF.Sigmoid, [dB[3]], [dGB])
                        tt("dve", U1[:], PB_[0][:], GA[:], ALU.mult, [dB[0], dGA], [dU1])
                        tt("dve", U2[:], PB_[1][:], GB[:], ALU.mult, [dB[1], dGB], [dU2])
                        tt("pool", ZT[qb][:, oc, :], U1[:], U2[:], ALU.add, [dU1, dU2], [dZT[qb]])
                    st_(zTd[:, :, sl_], ZT[qb][:], [dZT[qb]])
                end_phase()
        with ExitStack() as st:
            WST = [sbt(st, "WSTb%d" % i, [128, 8, 256], F32) for i in range(2)]; dWST = [Dep(), Dep()]
            WOUT = sbt(st, "WOUT", [128, 8, D], BF16); dWOUT = Dep()
            BC = sbt(st, "BC5", [128, 5, D], F32); dBC = Dep()
            ZC = [sbt(st, "ZC%d" % i, [128, 8, 512], BF16) for i in range(2)]; dZC = [Dep(), Dep()]
            XR = [sbt(st, "XR%d" % i, [128, D], F32) for i in range(2)]; dXR = [Dep(), Dep()]
            TMP = sbt(st, "TMP", [128, D], F32); dTMP = Dep()
            PRE = sbt(st, "PRE", [128, D], F32); dPRE = Dep()
            JNK = sbt(st, "JNK", [128, D], F32); dJNK = Dep()
            X1 = [sbt(st, "X1_%d" % i, [128, D], F32) for i in range(2)]; dX1 = [Dep(), Dep()]
            H2 = sbt(st, "H2", [128, D], F32); dH2 = Dep()
            H2T = [sbt(st, "H2T%d" % i, [128, 8, 128], BF16) for i in range(2)]; dH2T = [Dep(), Dep()]
            STA = sbt(st, "STA", [128, 8], F32); dSTA = Dep()
            sv = w_out.rearrange("(k p) n -> p k n", p=128)
            for c0 in range(0, D, 256):
                i = (c0 // 256) % 2
                ld(WST[i][:], sv[:, :, c0:c0 + 256], [dWST[i]])
                cp("dve", WOUT[:, :, c0:c0 + 256], WST[i][:], [dWST[i]], [dWOUT])
            for i, j in enumerate([0, 1, 2, 4, 5]):
                ld(BC[:, i, :], bctd[:, j, :], [dBC])

            def layer_norm(PREt, dPREt, gi, bi, OUTt, dOUTt, BCt, dBCt):
                act(JNK[:], PREt[:], ACTF.Identity, [dPREt], [dJNK, dSTA], accum=STA[:, 0:1])
                act(JNK[:], PREt[:], ACTF.Square, [dPREt], [dJNK, dSTA], accum=STA[:, 1:2])
                ts("dve", STA[:, 2:3], STA[:, 0:1], 1.0 / D, None, ALU.mult, None, [dSTA], [dSTA])
                tt("dve", STA[:, 3:4], STA[:, 2:3], STA[:, 2:3], ALU.mult, [dSTA], [dSTA])
                stt("dve", STA[:, 4:5], STA[:, 1:2], 1.0 / D, STA[:, 3:4], ALU.mult, ALU.subtract, [dSTA], [dSTA])
                act(STA[:, 5:6], STA[:, 4:5], ACTF.Sqrt, [dSTA], [dSTA], bias=EPS, scale=1.0)
                recip(STA[:, 6:7], STA[:, 5:6], [dSTA], [dSTA])
                ts("dve", OUTt[:], PREt[:], STA[:, 2:3], STA[:, 6:7], ALU.subtract, ALU.mult, [dPREt, dSTA], [dOUTt])
                tt("pool", OUTt[:], OUTt[:], BCt[:, gi, :], ALU.mult, [dOUTt, dBCt], [dOUTt])
                tt("pool", OUTt[:], OUTt[:], BCt[:, bi, :], ALU.add, [dOUTt, dBCt], [dOUTt])

            for qc in range(8):
                qb = qc % 2
                ld(ZC[qb][:], zTd[:, :, qc * 512:(qc + 1) * 512], [dZC[qb]])
                for t4 in range(4):
                    ti = qc * 4 + t4
                    xb = ti % 2
                    ld(XR[xb][:], x[ti * 128:(ti + 1) * 128, :], [dXR[xb]])
                    for hf in range(2):
                        for oc in range(8):
                            mm(PB_[hf][:], ZC[qb][:, oc, t4 * 128:(t4 + 1) * 128], WOUT[:, oc, hf * 512:(hf + 1) * 512],
                               oc == 0, oc == 7, [dZC[qb], dWOUT], [dB[hf]])
                        tt("dve", TMP[:, hf * 512:(hf + 1) * 512], PB_[hf][:], BC[:, 0, hf * 512:(hf + 1) * 512], ALU.mult,
                           [dB[hf], dBC], [dTMP])
                    stt("dve", PRE[:], XR[xb][:], ALPHA, TMP[:], ALU.mult, ALU.add, [dXR[xb], dTMP], [dPRE])
                    layer_norm(PRE, dPRE, 3, 4, X1[xb], dX1[xb], BC, dBC)
                    st_(x1d[ti * 128:(ti + 1) * 128, :], X1[xb][:], [dX1[xb]])
                    tt("dve", H2[:], X1[xb][:], BC[:, 2, :], ALU.mult, [dX1[xb], dBC], [dH2])
                    tt("pool", H2[:], H2[:], BC[:, 1, :], ALU.add, [dH2, dBC], [dH2])
                    for k in range(8):
                        bk = 2 + k // 4
                        tr(PB_[bk][:, (k % 4) * 128:(k % 4 + 1) * 128], H2[:, k * 128:(k + 1) * 128], IDENT[:], [dH2, dIDENT], [dB[bk]])
                    for g in range(2):
                        cp("act", H2T[xb][:, g * 4:(g + 1) * 4, :], PB_[2 + g][:].rearrange("p (a b) -> p a b", b=128), [dB[2 + g]], [dH2T[xb]])
                    st_(h2Td[:, :, ti * 128:(ti + 1) * 128], H2T[xb][:], [dH2T[xb]])
            end_phase()
        with ExitStack() as st:
            GG = sbt(st, "GG", [128, 128, 256], BF16); dGG = Dep()
            KEYSb = sbt(st, "KEYSb", [128, 16, 128], BF16); dKEYS = Dep()
            BC7 = sbt(st, "BC7", [128, 3, D], F32); dBC7 = Dep()
            H2C = [sbt(st, "H2C%d" % i, [128, 8, 256], BF16) for i in range(2)]; dH2C = [Dep(), Dep()]
            WQ = [sbt(st, "WQ%d" % i, [128, 8, 128], BF16) for i in range(2)]; dWQ = [Dep(), Dep()]
            QPT = sbt(st, "QPT", [128, 16, 256], BF16); dQPT = Dep()
            SCB = [sbt(st, "SCB%d" % i, [128, 2048], F32) for i in range(2)]; dSCB = [Dep(), Dep()]
            BUFA = sbt(st, "BUFA", [128, 2048], F32); dBUFA = Dep()
            BUFB = sbt(st, "BUFB", [128, 2048], F32); dBUFB = Dep()
            V16 = sbt(st, "V16", [128, 16, 16], F32); dV16 = Dep()
            IX = sbt(st, "IX", [128, 16, 16], U32); dIX = Dep()
            IXF = sbt(st, "IXF", [128, 16, 16], F32); dIXF = Dep()
            CV = sbt(st, "CV", [128, 8, 16], F32); dCV = Dep()
            CPi = sbt(st, "CPi", [128, 8, 16], U32); dCPi = Dep()
            PF = sbt(st, "PF", [128, 8, 16], F32); dPF = Dep()
            AF = sbt(st, "AF", [128, 8, 16], F32); dAF = Dep()
            BF = sbt(st, "BF", [128, 8, 16], F32); dBF = Dep()
            SLT = [sbt(st, "SLT%d" % i, [128, 3, 128], F32) for i in range(2)]; dSLT = [Dep(), Dep()]
            EG = sbt(st, "EG", [128, 8, 16], F32); dEG = Dep()
            SG = sbt(st, "SG", [128, 8], F32); dSG = Dep()
            TR3 = [sbt(st, "TR3_%d" % i, [128, 384], F32) for i in range(2)]; dTR3 = [Dep(), Dep()]
            OH1 = [sbt(st, "OH1_%d" % i, [128, 16, 128], BF16) for i in range(2)]; dOH1 = [Dep(), Dep()]
            OH2 = [sbt(st, "OH2_%d" % i, [128, 16, 128], BF16) for i in range(2)]; dOH2 = [Dep(), Dep()]
            NUB = 3
            UG = [sbt(st, "UG%d" % i, [128, 8, 256], BF16) for i in range(NUB)]; dUG = [Dep() for _ in range(NUB)]
            VG = [sbt(st, "VG%d" % i, [128, 2, D], BF16) for i in range(NUB)]; dVG = [Dep() for _ in range(NUB)]
            GE = [sbt(st, "GE%d" % i, [128, 256], BF16) for i in range(2)]; dGE = [Dep(), Dep()]
            WT = [sbt(st, "WT%d" % i, [128, 256], BF16) for i in range(2)]; dWT = [Dep(), Dep()]
            TMP = sbt(st, "TMP7", [128, D], F32); dTMP = Dep()
            OUTt = sbt(st, "OUTt", [128, D], F32); dOUTt = Dep()
            X1t = OUTt; dX1t = dOUTt
            STA = sbt(st, "STA7", [128, 8], F32); dSTA = Dep()
            dB4h = [Dep(), Dep()]
            DUM = sbt(st, "DUM", [128, 512], BF16); dDUM = Dep()
            THR16 = sbt(st, "THR16", [128, 16], F32); dTHR16 = Dep()
            ts("dve", THR16[:], IOTA[:, 0:16], 16.0, None, ALU.mult, None, [dIOTA], [dTHR16])
            fw.dma("pool", KEYSb[:].rearrange("p a b -> p (a b)"), keysT, writes=[dKEYS])
            for i, j in enumerate([3, 6, 7]):
                ld(BC7[:, i, :], bctd[:, j, :], [dBC7])
            wqv = wpqb.rearrange("(k p) n -> p k n", p=128)
            WKv = BUFB[:].rearrange("p (a b) -> p a b", b=128)
            OHA = BUFB[:].rearrange("p (h a b) -> p h a b", a=16, b=16); dOHA = dBUFB
            CAND = BUFA[:].rearrange("p (h a b) -> p h a b", a=16, b=16)
            CANDf = BUFA[:].rearrange("p (h c) -> p h c", c=256)
            CWf = BUFB[:].rearrange("p (h c) -> p h c", c=256)
            V16v = V16[:].rearrange("p (h m) a -> p h m a", m=2)
            IXFv = IXF[:].rearrange("p (h m) a -> p h m a", m=2)
            B16 = [128, 8, 16, 16]
            ohc = [0]; gcn = [0]; ugn = [0]

            def ln7(PREt, dPREt, OUTt_, dOUTt_):
                act(OUTt_[:], PREt[:], ACTF.Identity, [dPREt], [dOUTt_, dSTA], accum=STA[:, 0:1])
                act(OUTt_[:], PREt[:], ACTF.Square, [dPREt], [dOUTt_, dSTA], accum=STA[:, 1:2])
                ts("dve", STA[:, 2:3], STA[:, 0:1], 1.0 / D, None, ALU.mult, None, [dSTA], [dSTA])
                tt("dve", STA[:, 3:4], STA[:, 2:3], STA[:, 2:3], ALU.mult, [dSTA], [dSTA])
                stt("dve", STA[:, 4:5], STA[:, 1:2], 1.0 / D, STA[:, 3:4], ALU.mult, ALU.subtract, [dSTA], [dSTA])
                act(STA[:, 5:6], STA[:, 4:5], ACTF.Sqrt, [dSTA], [dSTA], bias=EPS, scale=1.0)
                recip(STA[:, 6:7], STA[:, 5:6], [dSTA], [dSTA])
                ts("dve", OUTt_[:], PREt[:], STA[:, 2:3], STA[:, 6:7], ALU.subtract, ALU.mult, [dPREt, dSTA], [dOUTt_])
                tt("pool", OUTt_[:], OUTt_[:], BC7[:, 1, :], ALU.mult, [dOUTt_, dBC7], [dOUTt_])
                tt("pool", OUTt_[:], OUTt_[:], BC7[:, 2, :], ALU.add, [dOUTt_, dBC7], [dOUTt_])

            def top16_batch(n, vals, idxs, src, wk, dvals, didx, dsrc, dwk):
                for g in range(n):
                    fw.op("dve", lambda e, g=g: e.max(out=vals(g)[:, 0:8], in_=src(g)), [dsrc], [dvals])
                for g in range(n):
                    fw.op("dve", lambda e, g=g: e.max_index(out=idxs(g)[:, 0:8], in_max=vals(g)[:, 0:8], in_values=src(g)), [dsrc, dvals], [didx])
                for g in range(n):
                    fw.op("dve", lambda e, g=g: e.match_replace(out=wk(g), in_to_replace=vals(g)[:, 0:8], in_values=src(g), imm_value=NEG),
                          [dsrc, dvals], [dwk])
                for g in range(n):
                    fw.op("dve", lambda e, g=g: e.max(out=vals(g)[:, 8:16], in_=wk(g)), [dwk], [dvals])
                for g in range(n):
                    fw.op("dve", lambda e, g=g: e.max_index(out=idxs(g)[:, 8:16], in_max=vals(g)[:, 8:16], in_values=wk(g)), [dwk, dvals], [didx])

            def stageA_front(c):
                cb = c % 2
                ld(H2C[cb][:], h2Td[:, :, c * 256:(c + 1) * 256], [dH2C[cb]])
                for hm in range(16):
                    ld(WQ[hm % 2][:], wqv[:, :, hm * 128:(hm + 1) * 128], [dWQ[hm % 2]])
                    r = (hm % 2) * 256
                    for k in range(8):
                        mm(PB_[6][:, r:r + 256], WQ[hm % 2][:, k, :], H2C[cb][:, k, :], k == 0, k == 7, [dWQ[hm % 2], dH2C[cb]], [dB[6]])
                    if hm % 2 == 1:
                        cp("act", QPT[:, hm - 1:hm + 1, :], PB_[6][:].rearrange("p (a b) -> p a b", b=256), [dB[6]], [dQPT])
                for t2 in range(2):
                    SC = SCB[t2][:].rearrange("p (a b) -> p a b", b=128)
                    for g in range(4):
                        for j in range(4):
                            hm = g * 4 + j
                            mm(PB_[7][:, j * 128:(j + 1) * 128], QPT[:, hm, t2 * 128:(t2 + 1) * 128], KEYSb[:, hm, :], True, True,
                               [dQPT, dKEYS], [dB[7]])
                        cp("act", SCB[t2][:, g * 512:(g + 1) * 512], PB_[7][:], [dB[7]], [dSCB[t2]])
                for t2 in range(2):
                    SC = SCB[t2][:].rearrange("p (a b) -> p a b", b=128)
                    top16_batch(16, lambda g: V16[:, g, :], lambda g: IX[:, g, :], lambda g, SC=SC: SC[:, g, :], lambda g: WKv[:, g, :],
                                dV16, dIX, dSCB[t2], dBUFB)
                    cp("dve", IXF[:], IX[:], [dIX], [dIXF])
                    tt("dve", CAND, V16v[:, :, 0, :].unsqueeze(3).to_broadcast(B16), V16v[:, :, 1, :].unsqueeze(2).to_broadcast(B16),
                       ALU.add, [dV16], [dBUFA])
                    top16_batch(8, lambda g: CV[:, g, :], lambda g: CPi[:, g, :], lambda g: CANDf[:, g, :], lambda g: CWf[:, g, :],
                                dCV, dCPi, dBUFA, dBUFB)
                    cp("dve", PF[:], CPi[:], [dCPi], [dPF])
                    tt("dve", OHA, PF[:].unsqueeze(3).to_broadcast(B16), THR16[:, 0:16].unsqueeze(1).unsqueeze(1).to_broadcast(B16),
                       ALU.is_ge, [dPF, dTHR16], [dOHA])
                    fw.op("dve", lambda e: e.tensor_reduce(AF[:], OHA, AX.X, ALU.add), [dOHA], [dAF])
                    ts("dve", AF[:], AF[:], -1.0, None, ALU.add, None, [dAF], [dAF])
                    stt("dve", BF[:], AF[:], -16.0, PF[:], ALU.mult, ALU.add, [dAF, dPF], [dBF])
                    iob = IOTA[:, 0:16].unsqueeze(1).unsqueeze(1).to_broadcast(B16)
                    for (XF, dXF, mi) in ((AF, dAF, 0), (BF, dBF, 1)):
                        tt("dve", OHA, XF[:].unsqueeze(3).to_broadcast(B16), iob, ALU.is_equal, [dXF, dIOTA], [dOHA])
                        tt("dve", OHA, OHA, IXFv[:, :, mi, :].unsqueeze(2).to_broadcast(B16), ALU.mult, [dOHA, dIXF], [dOHA])
                        fw.op("dve", lambda e, mi=mi, t2=t2: e.tensor_reduce(SLT[t2][:, mi, :].rearrange("p (a b) -> p a b", b=16), OHA, AX.X, ALU.add),
                              [dOHA], [dSLT[t2]])
                    tt("dve", EG[:], CV[:], CV[:, :, 0:1].to_broadcast([128, 8, 16]), ALU.subtract, [dCV], [dEG])
                    act(EG[:], EG[:], ACTF.Exp, [dEG], [dEG])
                    fw.op("dve", lambda e: e.reduce_sum(SG[:], EG[:], AX.X), [dEG], [dSG])
                    recip(SG[:], SG[:], [dSG], [dSG])
                    tt("dve", SLT[t2][:, 2, :].rearrange("p (a b) -> p a b", b=16), EG[:], SG[:].unsqueeze(2).to_broadcast([128, 8, 16]), ALU.mult,
                       [dEG, dSG], [dSLT[t2]])

            def stageA_back(c):
                for t2 in range(2):
                    for i in range(3):
                        tr(PB_[6][:, i * 128:(i + 1) * 128], SLT[t2][:, i, :], IDENT[:], [dSLT[t2], dIDENT], [dB[6]])
                    cp("act", TR3[t2][:], PB_[6][:, 0:384], [dB[6]], [dTR3[t2]])

            def stageB(c):
                for t2 in range(2):
                    T3 = TR3[t2]; dT3 = dTR3[t2]
                    for s in range(8):
                        ob = ohc[0] % 2; ohc[0] += 1
                        t0 = s * 16
                        B3 = [128, 16, 128]
                        iot = IOTA[:].unsqueeze(1).to_broadcast(B3)
                        tt("dve", OH1[ob][:], iot, T3[:, t0:t0 + 16].unsqueeze(2).to_broadcast(B3), ALU.is_equal,
                           [dIOTA, dT3], [dOH1[ob]])
                        tt("dve", OH1[ob][:], OH1[ob][:], T3[:, 256 + t0:256 + t0 + 16].unsqueeze(2).to_broadcast(B3), ALU.mult,
                           [dOH1[ob], dT3], [dOH1[ob]])
                        tt("dve", OH2[ob][:], iot, T3[:, 128 + t0:128 + t0 + 16].unsqueeze(2).to_broadcast(B3), ALU.is_equal,
                           [dIOTA, dT3], [dOH2[ob]])
                        for q4 in range(4):
                            gb = 5 if (gcn[0] % 2 == 0) else 7
                            gcn[0] += 1
                            for j in range(4):
                                tl = q4 * 4 + j
                                mm(PB_[gb][:, j * 128:(j + 1) * 128], OH1[ob][:, tl, :], OH2[ob][:, tl, :], True, True,
                                   [dOH1[ob], dOH2[ob]], [dB[gb]])
                            tg = t2 * 128 + t0 + q4 * 4
                            cp("act", GG[:, :, tg:tg + 4],
                               PB_[gb][:].rearrange("p (t i) -> p i t", i=128), [dB[gb]], [dGG])
                            for _ in range(2):
                                cp("act", DUM[:], PB_[gb][:], [dB[gb]], [dDUM])

            def dense(c):
                cb = c % 2

                def stage1(i2):
                    gi = i2 // 2; bl = i2 % 2
                    if bl == 0:
                        ub = ugn[0] % NUB; ugn[0] += 1
                        ld(UG[ub][:].rearrange("p k n -> p (k n)"), uTb_r[gi], [dUG[ub]])
                        ld(VG[ub][:], vRb[gi * 256:(gi + 1) * 256, :].rearrange("(b p) n -> p b n", p=128), [dVG[ub]])
                    ub = (ugn[0] - 1) % NUB
                    r = (i2 % 2) * 256
                    for k in range(8):
                        mm(PB_[4][:, r:r + 256], UG[ub][:, k, bl * 128:(bl + 1) * 128], H2C[cb][:, k, :], k == 0, k == 7,
                           [dUG[ub], dH2C[cb]], [dB4h[i2 % 2]])
                    return ub
                ubs = {}
                ubs[0] = stage1(0)
                for i2 in range(128):
                    if i2 + 1 < 128:
                        ubs[i2 + 1] = stage1(i2 + 1)
                    bl = i2 % 2; ub = ubs[i2]
                    r = (i2 % 2) * 256
                    act(GE[i2 % 2][:], PB_[4][:, r:r + 256], ACTF.Gelu_apprx_tanh, [dB4h[i2 % 2]], [dGE[i2 % 2]])
                    tt("pool", WT[i2 % 2][:], GE[i2 % 2][:], GG[:, i2, :], ALU.mult, [dGE[i2 % 2], dGG], [dWT[i2 % 2]])
                    for t2 in range(2):
                        for hf in range(2):
                            bk = t2 * 2 + hf
                            mm(PB_[bk][:], WT[i2 % 2][:, t2 * 128:(t2 + 1) * 128], VG[ub][:, bl, hf * 512:(hf + 1) * 512],
                               i2 == 0, i2 == 127, [dWT[i2 % 2], dVG[ub]], [dB[bk]])

            def epilogue(c):
                for t2 in range(2):
                    ti = c * 2 + t2
                    ld(X1t[:], x1d[ti * 128:(ti + 1) * 128, :], [dX1t])
                    for hf in range(2):
                        tt("dve", TMP[:, hf * 512:(hf + 1) * 512], PB_[t2 * 2 + hf][:], BC7[:, 0, hf * 512:(hf + 1) * 512], ALU.mult,
                           [dB[t2 * 2 + hf], dBC7], [dTMP])
                    stt("dve", TMP[:], X1t[:], ALPHA, TMP[:], ALU.mult, ALU.add, [dX1t, dTMP], [dTMP])
                    ln7(TMP, dTMP, OUTt, dOUTt)
                    st_(out[ti * 128:(ti + 1) * 128, :], OUTt[:], [dOUTt])

            stageA_front(0); stageA_back(0); stageB(0)
            for c in range(NCH):
                if c + 1 < NCH:
                    stageA_front(c + 1)
                dense(c)
                epilogue(c)
                if c + 1 < NCH:
                    stageA_back(c + 1)
                    stageB(c + 1)
            fw.finish()
            end_phase()
    return nc


def _prep_shared(inp):
    f = np.float32
    w_in = np.asarray(inp["w_in"][0], f)
    perm64 = np.concatenate([np.arange(32, 64), np.arange(0, 32)])
    perm128 = np.concatenate([perm64, 64 + perm64])
    Wq = w_in[:, 0:1024]; Wk = w_in[:, 3328:4352]; Wv = w_in[:, 4352:5376]
    w_da = np.stack([np.concatenate([Wq[:, h * 128:(h + 1) * 128], Wq[:, h * 128 + perm128],
                                     Wk[:, h * 128:(h + 1) * 128], Wk[:, h * 128 + perm128],
                                     Wv[:, h * 128:(h + 1) * 128]], axis=1) for h in range(8)])
    w_lat = np.concatenate([w_in[:, 1024:1280], w_in[:, 5376:5632], w_in[:, 5632:5696], w_in[:, 5632 + perm64]], axis=1)
    wq = np.asarray(inp["w_q_up"][0], f); wkv = np.asarray(inp["w_kv_up"][0], f)
    w_mla = np.stack([np.concatenate([wq[:, h * 192:h * 192 + 128], wq[:, h * 192 + 128:h * 192 + 192],
                                      wq[:, h * 192 + 128 + perm64], wkv[:, h * 256:h * 256 + 128],
                                      wkv[:, h * 256 + 128:h * 256 + 256]], axis=1) for h in range(8)])
    rows4 = np.concatenate([inp["ln1_g"][0], inp["ln1_b"][0], inp["ln2_g"][0], inp["ln2_b"][0]]).astype(f)[None]
    lamrow = np.concatenate([inp["lambda_q1"][0], inp["lambda_k1"][0], inp["lambda_q2"][0], inp["lambda_k2"][0]]).astype(f)[None]
    colsm = np.concatenate([np.asarray(inp["q_norm_g"][0], f).reshape(2, 128).T, np.asarray(inp["kv_norm_g"][0], f).reshape(2, 128).T,
                            np.asarray(inp["subln_g"][0], f).reshape(128, 1)], axis=1)
    quarter = 16
    inv = (10000.0 ** (-np.arange(quarter, dtype=f) / quarter)).astype(f)
    tpos = np.arange(T)
    ang = np.concatenate([(tpos // 64).astype(f)[:, None] * inv, (tpos % 64).astype(f)[:, None] * inv], axis=1).astype(f)
    p = np.arange(128)
    cosT = np.cos(ang).astype(f).T[p % 32]
    sinT = np.sin(ang).astype(f).T[p % 32]
    sign = np.where((p % 64) < 32, -1.0, 1.0).astype(f)[:, None]
    tabC = np.concatenate([np.ones((128, NCTX), f), cosT], axis=1)
    tabS = np.concatenate([np.zeros((128, NCTX), f), sinT * sign], axis=1)
    b_mod = np.asarray(inp["b_mod"][0], f)
    keys = np.asarray(inp["peer_keys"][0], f)
    keysT = np.ascontiguousarray(keys.transpose(3, 0, 1, 2).reshape(128, 16 * 128))
    U = np.asarray(inp["peer_u"][0], f); V = np.asarray(inp["peer_v"][0], f)
    uT0 = U.reshape(128, 128, 1024).transpose(2, 1, 0).reshape(8, 128, 64, 256)
    uT = np.ascontiguousarray(uT0.transpose(2, 1, 0, 3).reshape(64, 128, 2048))
    vR = np.ascontiguousarray(V.reshape(128, 128, 1024).transpose(1, 0, 2).reshape(16384, 1024))
    sh = {
        "w_mod": np.ascontiguousarray(inp["w_mod"][0], f), "b_row": b_mod[None].copy(),
        "b_col": np.ascontiguousarray(b_mod.reshape(48, 128).T),
        "w_lat": np.ascontiguousarray(w_lat), "w_da": np.ascontiguousarray(w_da), "w_mla": np.ascontiguousarray(w_mla),
        "w_gate": np.ascontiguousarray(w_in[:, 1280:3328]),
        "w_pa": np.ascontiguousarray(inp["w_pa"][0], f), "w_pb": np.ascontiguousarray(inp["w_pb"][0], f),
        "w_out": np.ascontiguousarray(inp["w_out"][0], f),
        "rows4": rows4, "lamrow": lamrow, "colsm": np.ascontiguousarray(colsm),
        "tabC": np.ascontiguousarray(tabC), "tabS": np.ascontiguousarray(tabS),
        "ident": np.eye(128, dtype=f), "iota": np.ascontiguousarray(np.tile(np.arange(128, dtype=f)[None], (128, 1))),
        "w_pq": np.ascontiguousarray(inp["w_pq"][0], f), "keysT": keysT, "uT": uT, "vR": vR,
    }
    return sh


def _in_maps(inp):
    sh = _prep_shared(inp)
    f = np.float32
    cctx = np.asarray(inp["c_ctx"], f).reshape(8, 128).T
    maps = []
    for b in range(8):
        m = dict(sh)
        m["x"] = np.ascontiguousarray(inp["x"][b], f)
        m["ctx"] = np.ascontiguousarray(inp["ctx"][b], f)
        m["cT"] = np.ascontiguousarray(np.concatenate([np.asarray(inp["c"][b], f).reshape(8, 128).T, cctx], axis=1))
        maps.append(m)
    return maps


def kernel(**inputs):
    inp = {k: np.asarray(v) for k, v in inputs.items()}
    nc = build_program()
    res = run_bass_kernel_spmd(nc, _in_maps(inp), core_ids=list(range(8)))
    return np.stack([np.asarray(r["out"], np.float32) for r in res.results], axis=0)
```
